# Optimizing a Trainium2 kernel written in Bass

```python
import math
import jax, jax.numpy as jnp
from jax import lax
import numpy as np

D_MODEL = 1024
BATCH = 16
SEQ = 2048
DEPTH = 1

HEAD_DIM = 64
N_HEADS_A = D_MODEL // (2 * HEAD_DIM)
N_HEADS_B = D_MODEL // (2 * HEAD_DIM)
WIDTH_A = N_HEADS_A * HEAD_DIM
WIDTH_B = N_HEADS_B * HEAD_DIM
MIX_WIDTH = WIDTH_A + WIDTH_B
N_IDX_HEADS = 8
IDX_DIM = 64
TOPK_MAX = 256
D_FF = 2816
N_BUCKETS = 32
MAX_DISTANCE = 128
SPARSE_BLOCK = 32
SB_BLOCK = 128
N_SUBLAYERS = 3
N_MOD = 3 * N_SUBLAYERS
EPS = 1e-6

IN_SIZES = [WIDTH_A, WIDTH_A, WIDTH_A, N_IDX_HEADS * IDX_DIM, IDX_DIM, N_IDX_HEADS,
            WIDTH_B, WIDTH_B, WIDTH_B]
IN_COLS = sum(IN_SIZES)
IN_SPLITS = list(np.cumsum(IN_SIZES)[:-1])

kernel_name = "hybrid_dsa_stickbreaking_macaron_block"


def rmsnorm(x, g):
    xf = x.astype(jnp.float32)
    y = xf * lax.rsqrt(jnp.mean(xf * xf, axis=-1, keepdims=True) + EPS)
    return (y * g.astype(jnp.float32)).astype(x.dtype)


def modulate(x, shift, scale):
    return x * (1 + scale[:, None, :]) + shift[:, None, :]


def swiglu(x, w_gate, w_up, w_down):
    return (jax.nn.silu(x @ w_gate) * (x @ w_up)) @ w_down


def t5_bucket(n):
    n = jnp.maximum(n, 0)
    max_exact = N_BUCKETS // 2
    nf = jnp.maximum(n, 1).astype(jnp.float32)
    large = max_exact + (jnp.log(nf / max_exact) / math.log(MAX_DISTANCE / max_exact)
                         * (N_BUCKETS - max_exact)).astype(jnp.int32)
    large = jnp.minimum(large, N_BUCKETS - 1)
    return jnp.where(n < max_exact, n, large)


def dsa_sparse_attention(q, k, v, q_idx, k_idx, w_idx, rel_bias):
    B, S, H, D = q.shape
    topk = min(TOPK_MAX, S // 4)
    nb = S // SPARSE_BLOCK
    key_pos = jnp.arange(S)

    def blocks(a):
        return jnp.swapaxes(a.reshape(B, nb, SPARSE_BLOCK, *a.shape[2:]), 0, 1)

    def one(args):
        qb, qib, wb, t0 = args
        tpos = t0 + jnp.arange(SPARSE_BLOCK)
        sc = jax.nn.relu(jnp.einsum('bthd,bsd->bths', qib, k_idx).astype(jnp.float32))
        score = jnp.einsum('bths,bth->bts', sc, wb.astype(jnp.float32))
        causal = key_pos[None, :] <= tpos[:, None]
        score = jnp.where(causal[None], score, -jnp.inf)
        _, idx = lax.top_k(score, topk)
        kg = jax.vmap(lambda kk, ii: kk[ii])(k, idx)
        vg = jax.vmap(lambda vv, ii: vv[ii])(v, idx)
        dist = tpos[None, :, None] - idx
        valid = dist >= 0
        bias = jnp.swapaxes(rel_bias[t5_bucket(dist)], -1, -2)
        logits = (jnp.einsum('bthd,btkhd->bthk', qb, kg).astype(jnp.float32) * (D ** -0.5)
                  + bias.astype(jnp.float32))
        logits = jnp.where(valid[:, :, None, :], logits, -jnp.inf)
        p = jax.nn.softmax(logits, axis=-1)
        return jnp.einsum('bthk,btkhd->bthd', p.astype(v.dtype), vg)

    starts = jnp.arange(nb) * SPARSE_BLOCK
    out = lax.map(one, (blocks(q), blocks(q_idx), blocks(w_idx), starts))
    return jnp.swapaxes(out, 0, 1).reshape(B, S, H * D)


def stick_breaking_attention(q, k, v):
    B, S, H, D = q.shape
    nb = S // SB_BLOCK
    key_pos = jnp.arange(S)
    q_blocks = jnp.swapaxes(q.reshape(B, nb, SB_BLOCK, H, D), 0, 1)

    def one(args):
        qb, t0 = args
        tpos = t0 + jnp.arange(SB_BLOCK)
        z = jnp.einsum('bthd,bshd->bhts', qb, k).astype(jnp.float32) * (D ** -0.5)
        mask = (key_pos[None, :] < tpos[:, None])[None, None]
        u = jnp.where(mask, jax.nn.log_sigmoid(-z), 0.0)
        between = lax.cumsum(u, axis=3, reverse=True) - u
        a = jnp.where(mask, jnp.exp(jax.nn.log_sigmoid(z) + between), 0.0)
        return jnp.einsum('bhts,bshd->bthd', a.astype(v.dtype), v)

    starts = jnp.arange(nb) * SB_BLOCK
    out = lax.map(one, (q_blocks, starts))
    return jnp.swapaxes(out, 0, 1).reshape(B, S, H * D)


def setup_inputs(seed: int = 0) -> dict:
    key = jax.random.key(seed)
    ks = jax.random.split(key, 20)
    f32 = jnp.float32
    nrm = lambda k, shape, s: jax.random.normal(k, shape, f32) * s
    return {
        "x": nrm(ks[0], (BATCH, SEQ, D_MODEL), 1.0),
        "c": nrm(ks[1], (BATCH, D_MODEL), 1.0),
        "w_ada": nrm(ks[2], (DEPTH, D_MODEL, N_MOD * D_MODEL), 0.5 * D_MODEL ** -0.5),
        "b_ada": nrm(ks[3], (DEPTH, N_MOD * D_MODEL), 0.02),
        "g_pre": 1.0 + nrm(ks[4], (DEPTH, N_SUBLAYERS, D_MODEL), 0.05),
        "g_post": 1.0 + nrm(ks[5], (DEPTH, N_SUBLAYERS, D_MODEL), 0.05),
        "w_ffn_gate": nrm(ks[6], (DEPTH, 2, D_MODEL, D_FF), D_MODEL ** -0.5),
        "w_ffn_up": nrm(ks[7], (DEPTH, 2, D_MODEL, D_FF), D_MODEL ** -0.5),
        "w_ffn_down": nrm(ks[8], (DEPTH, 2, D_FF, D_MODEL), D_FF ** -0.5),
        "w_in": nrm(ks[9], (DEPTH, D_MODEL, IN_COLS), D_MODEL ** -0.5),
        "g_kidx": 1.0 + nrm(ks[10], (DEPTH, IDX_DIM), 0.05),
        "rel_bias": nrm(ks[11], (N_BUCKETS, N_HEADS_A), 0.5),
        "g_out_a": 1.0 + nrm(ks[12], (DEPTH, WIDTH_A), 0.05),
        "g_out_b": 1.0 + nrm(ks[13], (DEPTH, WIDTH_B), 0.05),
        "w_out": nrm(ks[14], (DEPTH, MIX_WIDTH, D_MODEL), MIX_WIDTH ** -0.5),
    }


def reference(x, c, w_ada, b_ada, g_pre, g_post, w_ffn_gate, w_ffn_up, w_ffn_down,
              w_in, g_kidx, rel_bias, g_out_a, g_out_b, w_out):
    B, S, _ = x.shape
    h = x
    for l in range(DEPTH):
        mods = (jax.nn.silu(c) @ w_ada[l] + b_ada[l]).reshape(B, N_MOD, D_MODEL)
        shift = lambda i: mods[:, 3 * i]
        scale = lambda i: mods[:, 3 * i + 1]
        gate = lambda i: mods[:, 3 * i + 2][:, None, :]

        n = modulate(rmsnorm(h, g_pre[l, 0]), shift(0), scale(0))
        f = swiglu(n, w_ffn_gate[l, 0], w_ffn_up[l, 0], w_ffn_down[l, 0])
        h = h + 0.5 * gate(0) * rmsnorm(f, g_post[l, 0])

        n = modulate(rmsnorm(h, g_pre[l, 1]), shift(1), scale(1))
        p = n @ w_in[l]
        qa, ka, va, qi, ki, wi, qb, kb, vb = jnp.split(p, IN_SPLITS, axis=-1)
        qa = qa.reshape(B, S, N_HEADS_A, HEAD_DIM)
        ka = ka.reshape(B, S, N_HEADS_A, HEAD_DIM)
        va = va.reshape(B, S, N_HEADS_A, HEAD_DIM)
        qi = qi.reshape(B, S, N_IDX_HEADS, IDX_DIM)
        ki = rmsnorm(ki, g_kidx[l])
        wi = wi * ((N_IDX_HEADS * IDX_DIM) ** -0.5)
        qb = qb.reshape(B, S, N_HEADS_B, HEAD_DIM)
        kb = kb.reshape(B, S, N_HEADS_B, HEAD_DIM)
        vb = vb.reshape(B, S, N_HEADS_B, HEAD_DIM)

        oa = rmsnorm(dsa_sparse_attention(qa, ka, va, qi, ki, wi, rel_bias), g_out_a[l])
        ob = rmsnorm(stick_breaking_attention(qb, kb, vb), g_out_b[l])
        o = jnp.concatenate([oa, ob], axis=-1) @ w_out[l]
        h = h + gate(1) * rmsnorm(o, g_post[l, 1])

        n = modulate(rmsnorm(h, g_pre[l, 2]), shift(2), scale(2))
        f = swiglu(n, w_ffn_gate[l, 1], w_ffn_up[l, 1], w_ffn_down[l, 1])
        h = h + 0.5 * gate(2) * rmsnorm(f, g_post[l, 2])
    return h
```

```python
import numpy as np
import ml_dtypes
from contextlib import ExitStack
import concourse.bass as bass
import concourse.mybir as mybir
from concourse.bass_utils import run_bass_kernel_spmd

F32 = mybir.dt.float32
BF16 = mybir.dt.bfloat16
AF = mybir.ActivationFunctionType
ALU = mybir.AluOpType
AX = mybir.AxisListType

P = 128
D = 1024
S = 2048
NBC = 2
DFF = 2816
NFF = 22
KC = 8
TT = 1024
NSUB = TT // P
NTILE = S // TT
EPS = 1e-6
NIT = 18
TOPK = 256
NEG = -30000.0
WIN_COLS = 3656
N_CORES = 8


class Tl:
    __slots__ = ("name", "w", "r", "dsem", "dcnt")

    def __init__(self, name):
        self.name = name
        self.w = None
        self.r = {}
        self.dsem = None
        self.dcnt = 0


class Prog:
    ENG = ("pe", "act", "dve", "pool", "sp")

    def __init__(self, nc, stack):
        self.nc = nc
        self.stack = stack
        self.sems = []
        self.esid = {}
        for e in self.ENG:
            self.esid[e] = self.newsem("s_" + e)
        self.cnt = {e: 0 for e in self.ENG}
        self.q = {e: [] for e in self.ENG}
        self.waited = {e: {} for e in self.ENG}
        self.dtiles = []
        self.nops = 0

    def newsem(self, name):
        name = "%s_%d" % (name, len(self.sems))
        h = self.stack.enter_context(self.nc.semaphore(name))
        self.sems.append(h)
        return len(self.sems) - 1

    @staticmethod
    def _add(d, sv):
        if sv is None:
            return
        s, v = sv
        if d.get(s, 0) < v:
            d[s] = v

    def _collect(self, reads, writes):
        d = {}
        for t in reads:
            self._add(d, t.w)
        for t in writes:
            self._add(d, t.w)
            for s, v in t.r.items():
                self._add(d, (s, v))
        return d

    def _emit(self, eng, d, fn, sid, inc):
        w = self.waited[eng]
        waits = []
        for s, v in d.items():
            if w.get(s, 0) < v:
                w[s] = v
                waits.append((s, v))
        self.q[eng].append((waits, fn, sid, inc))
        self.nops += 1

    def _mark(self, tk, reads, writes):
        for t in writes:
            t.w = tk
            t.r = {}
        for t in reads:
            if t not in writes:
                if t.r.get(tk[0], 0) < tk[1]:
                    t.r[tk[0]] = tk[1]

    def op(self, eng, fn, reads=(), writes=()):
        d = self._collect(reads, writes)
        self.cnt[eng] += 1
        tk = (self.esid[eng], self.cnt[eng])
        self._emit(eng, d, fn, tk[0], 1)
        self._mark(tk, reads, writes)
        return tk

    def dma(self, eng, fn, semtile, reads=(), writes=()):
        d = self._collect(reads, writes)
        if semtile.dsem is None:
            semtile.dsem = self.newsem("d_" + semtile.name)
            self.dtiles.append(semtile)
        if semtile.dcnt > 0:
            self._add(d, (semtile.dsem, semtile.dcnt))
        semtile.dcnt += 16
        tk = (semtile.dsem, semtile.dcnt)
        self._emit(eng, d, fn, tk[0], 16)
        self._mark(tk, reads, writes)
        return tk

    def barrier(self):
        d = {}
        for e in self.ENG:
            if self.cnt[e] > 0:
                d[self.esid[e]] = self.cnt[e]
        for t in self.dtiles:
            d[t.dsem] = t.dcnt
        for e in self.ENG:
            self._emit(e, dict(d), None, None, 0)

    def run(self, e, name):
        sems = self.sems
        for waits, fn, sid, inc in self.q[name]:
            for s, v in waits:
                e.wait_ge(sems[s], v)
            if fn is not None:
                ins = fn(e)
                ins.then_inc(sems[sid], inc)


_DBGNAMES = []


class Arena:
    def __init__(self, nc, base, size):
        self.nc = nc
        self.base = base
        self.size = size
        self.off = 0
        self.uid = 0
        self.peak = 0

    def reset(self):
        self.off = 0

    def alloc(self, name, shape, dtype):
        esz = 2 if dtype == BF16 else 4
        n = esz
        for s in shape[1:]:
            n *= s
        addr = (self.off + 63) // 64 * 64
        self.off = addr + n
        self.peak = max(self.peak, self.off)
        assert self.off <= self.size, (name, self.off, self.size)
        self.uid += 1
        h = self.nc.alloc_sbuf_tensor_at("%s_%d" % (name, self.uid), list(shape), dtype,
                                         offset=self.base + addr)
        if name in ("mcol", "Sb0", "Sb1", "NM", "NMT0", "NMT1", "steps0", "steps1") or name.startswith("small"):
            _DBGNAMES.append(h.name)
        return h.ap()


class Buf:
    __slots__ = ("ap", "t")

    def __init__(self, ap, name):
        self.ap = ap
        self.t = Tl(name)


def build_program(debug=None):
    nc = bass.Bass("TRN2", target_bir_lowering=False)
    stack = ExitStack()
    p = Prog(nc, stack)

    def din(name, shape, dt=F32):
        return nc.dram_tensor(name, list(shape), dt, kind="ExternalInput").ap()

    def dint(name, shape, dt=F32):
        kind = "ExternalOutput" if (debug and debug.get("_expose")) else "Internal"
        return nc.dram_tensor(name, list(shape), dt, kind=kind).ap()

    x_d = din("x", [NBC, S, D])
    cT_d = din("cT", [P, KC, NBC])
    wada_d = din("wada", [18, P, KC, 512])
    bada_d = din("bada", [9 * D])
    gpreT_d = din("gpreT", [P, 3, KC])
    gpost_d = din("gpost", [3, D])
    wgu_d = [din("wgu%d" % f, [NFF, P, 2, KC, P]) for f in range(2)]
    wd_d = [din("wd%d" % f, [P, NFF, D]) for f in range(2)]
    winfm_d = din("winfm", [8, P, 2, KC, P])
    winv_d = din("winv", [4, P, KC, 256])
    winqi_d = din("winqi", [2, P, KC, 256])
    winkw_d = din("winkw", [P, KC, 72])
    wout_d = din("wout", [P, KC, D])
    gk_d = din("gk", [P, 64])
    goutT_d = din("goutT", [P, KC])
    traw_d = din("traw", [P, 2, 8, P])
    b31_d = din("b31", [P, 8])
    identb_d = din("identb", [P, P], BF16)
    identf_d = din("identf", [P, P])
    negtri_d = din("negtri", [P, P])
    nmtc_d = din("nmtc", [P, P], BF16)
    negdiag_d = din("negdiag", [P, P])
    sbmask_d = din("sbmask", [P, 4, 512], BF16)
    pow2_d = din("pow2", [P, NIT])
    selb_d = din("selb", [2, 2, P])
    selh_d = din("selh", [P, 2, P], BF16)
    out_d = nc.dram_tensor("out", [NBC, S, D], F32, kind="ExternalOutput").ap()

    h1_s = dint("h1_s", [NBC, S, D])
    h2_s = dint("h2_s", [NBC, S, D])
    o_s = dint("o_s", [NBC, S, D])
    grow_s = dint("grow_s", [NBC, 3, D])
    qkT_s = dint("qkT_s", [NBC, 4, 4, P, S], BF16)
    va_s = dint("va_s", [NBC, S, 8, 65], BF16)
    vb_s = dint("vb_s", [NBC, S, 512], BF16)
    qiT_s = dint("qiT_s", [NBC, 4, P, S])
    kiT_s = dint("kiT_s", [NBC, 64, S])
    sgn_s = dint("sgn_s", [NBC, S, 8])

    dbg = {}
    if debug:
        for name, shape in debug.items():
            if name.startswith("_"):
                continue
            dbg[name] = nc.dram_tensor("dbg_" + name, list(shape), F32, kind="ExternalOutput").ap()

    T_h1 = [[Tl("h1_%d_%d" % (b, r)) for r in range(S // P)] for b in range(NBC)]
    T_h2 = [[Tl("h2_%d_%d" % (b, r)) for r in range(S // P)] for b in range(NBC)]
    T_o = [[Tl("o_%d_%d" % (b, r)) for r in range(S // P)] for b in range(NBC)]
    T_att = [Tl("att_%d" % b) for b in range(NBC)]
    T_grow = Tl("grow")
    T_out = Tl("out")

    arena_bytes = 204 * 1024
    arena_h = nc.alloc_sbuf_tensor("arena", [P, arena_bytes // 4], F32)
    base = nc.lookup_mloc(arena_h).addr
    pers = Arena(nc, base, 10 * 1024)
    ar = Arena(nc, base + 10 * 1024, arena_bytes - 10 * 1024)

    ps_h = nc.alloc_psum_tensor("ps", [P, 8, 512], F32)
    ps = ps_h.ap()
    pb = [Tl("pb%d" % i) for i in range(8)]

    def pbf(i):
        return ps[:, i, :].bitcast(BF16)

    identb = Buf(pers.alloc("identb", [P, P], BF16), "identb")
    identf = Buf(pers.alloc("identf", [P, P], F32), "identf")
    zerosb = Buf(pers.alloc("zerosb", [P, 512], BF16), "zerosb")
    negtri = Buf(pers.alloc("negtri", [P, P], F32), "negtri")
    negones = Buf(pers.alloc("negones", [P, P], F32), "negones")
    nmtc = Buf(pers.alloc("nmtc", [P, P], BF16), "nmtc")
    negdiag = Buf(pers.alloc("negdiag", [P, P], F32), "negdiag")
    pow2 = Buf(pers.alloc("pow2", [P, NIT], F32), "pow2")
    selh = Buf(pers.alloc("selh", [P, 2, P], BF16), "selh")
    gk = Buf(pers.alloc("gk", [P, 64], F32), "gk")
    goutT = Buf(pers.alloc("goutT", [P, KC], F32), "goutT")
    b31 = Buf(pers.alloc("b31", [P, 8], F32), "b31")
    Acol = Buf(pers.alloc("Acol", [P, 3, NBC, KC], F32), "Acol")
    Scol = Buf(pers.alloc("Scol", [P, 3, NBC, KC], F32), "Scol")
    neghalf = Buf(pers.alloc("neghalf", [P, 1], F32), "neghalf")
    sgn_all = Buf(pers.alloc("sgn_all", [P, NBC, 16, 8], F32), "sgn_all")
    smallr = [Buf(pers.alloc("small%d" % i, [P, 8], F32), "small%d" % i) for i in range(32)]
    small_i = [0]

    def small():
        small_i[0] += 1
        return smallr[small_i[0] % len(smallr)]

    def ld(buf, src, eng="sp"):
        p.dma(eng, lambda e, o=buf.ap, i=src: e.dma_start(out=o, in_=i), buf.t, writes=[buf.t])

    for bf, src in ((identb, identb_d), (identf, identf_d), (negtri, negtri_d), (nmtc, nmtc_d),
                    (negdiag, negdiag_d), (pow2, pow2_d), (selh, selh_d), (gk, gk_d),
                    (goutT, goutT_d), (b31, b31_d)):
        ld(bf, src)
    p.op("dve", lambda e: e.memset(zerosb.ap, 0.0), writes=[zerosb.t])
    p.op("dve", lambda e: e.memset(negones.ap, -1.0), writes=[negones.t])
    p.op("dve", lambda e: e.memset(neghalf.ap, -0.5), writes=[neghalf.t])

    ar.reset()
    cT = Buf(ar.alloc("cT", [P, KC, NBC], F32), "cT")
    siluT = Buf(ar.alloc("siluT", [P, KC, NBC], F32), "siluT")
    wada = [Buf(ar.alloc("wada%d" % i, [P, KC, 512], F32), "wada%d" % i) for i in range(2)]
    mods = Buf(ar.alloc("mods", [2, 9 * D], F32), "mods")
    bada2 = Buf(ar.alloc("bada2", [2, 9 * D], F32), "bada2")
    gpost2 = Buf(ar.alloc("gpost2", [2, 3, D], F32), "gpost2")
    grow = Buf(ar.alloc("grow", [2, 3, D], F32), "grow")
    selb = Buf(ar.alloc("selb", [2, 2, P], F32), "selb")
    gpreT = Buf(ar.alloc("gpreT", [P, 3, KC], F32), "gpreT")
    modT = Buf(ar.alloc("modT", [P, 48, 2], F32), "modT")

    ld(cT, cT_d)
    ld(gpreT, gpreT_d)
    ld(selb, selb_d)
    p.dma("sp", lambda e: e.dma_start(out=bada2.ap, in_=bada_d.partition_broadcast(2)), bada2.t,
          writes=[bada2.t])
    p.dma("sp", lambda e: e.dma_start(out=gpost2.ap.rearrange("p a d -> p (a d)"),
                                      in_=gpost_d.rearrange("a d -> (a d)").partition_broadcast(2)),
          gpost2.t, writes=[gpost2.t])
    p.op("act", lambda e: e.activation(siluT.ap, cT.ap, AF.Silu), reads=[cT.t], writes=[siluT.t])
    for g in range(18):
        wb = wada[g % 2]
        ld(wb, wada_d[g])
        bk = g % 2

        def fn(e, wb=wb, bk=bk):
            r = None
            for k in range(KC):
                r = e.matmul(ps[0:2, bk, :], siluT.ap[:, k, :], wb.ap[:, k, :], start=(k == 0), stop=(k == KC - 1))
            return r
        p.op("pe", fn, reads=[wb.t, siluT.t], writes=[pb[bk]])
        p.op("dve", lambda e, g=g, bk=bk: e.tensor_tensor(mods.ap[:, g * 512:(g + 1) * 512], ps[0:2, bk, :],
                                                           bada2.ap[:, g * 512:(g + 1) * 512], ALU.add),
             reads=[pb[bk], bada2.t], writes=[mods.t])
    modT_ps = ps[:, 2, 0:96].rearrange("p (c b) -> p c b", b=2)

    def fn_modT(e):
        r = None
        for i in range(3):
            for which in range(2):
                for k in range(KC):
                    off = (3 * i + which) * D + k * P
                    idx = (i * 2 + which) * KC + k
                    r = e.matmul(modT_ps[:, idx, :], mods.ap[0:2, off:off + P], identf.ap[0:2, 0:2],
                                 start=True, stop=True)
        return r
    p.op("pe", fn_modT, reads=[mods.t, identf.t], writes=[pb[2]])
    p.op("dve", lambda e: e.tensor_copy(modT.ap, modT_ps), reads=[pb[2]], writes=[modT.t])
    modT_v = modT.ap.rearrange("p (i w k) b -> p i w k b", i=3, w=2)
    for i in range(3):
        for b in range(NBC):
            p.op("dve", lambda e, i=i, b=b: e.tensor_copy(Scol.ap[:, i, b, :], modT_v[:, i, 0, :, b]),
                 reads=[modT.t], writes=[Scol.t])
            p.op("dve", lambda e, i=i, b=b: e.scalar_tensor_tensor(
                Acol.ap[:, i, b, :], modT_v[:, i, 1, :, b], 1.0, gpreT.ap[:, i, :], ALU.add, ALU.mult),
                reads=[modT.t, gpreT.t], writes=[Acol.t])
    for i in range(3):
        coef = 1.0 if i == 1 else 0.5
        p.op("dve", lambda e, i=i, coef=coef: e.scalar_tensor_tensor(
            grow.ap[:, i, :], mods.ap[:, (3 * i + 2) * D:(3 * i + 3) * D], coef, gpost2.ap[:, i, :],
            ALU.mult, ALU.mult), reads=[mods.t, gpost2.t], writes=[grow.t])
    p.dma("sp", lambda e: e.dma_start(out=grow_s, in_=grow.ap), grow.t, reads=[grow.t], writes=[T_grow])
    if "mods" in dbg:
        p.dma("sp", lambda e: e.dma_start(out=dbg["mods"], in_=mods.ap), mods.t, reads=[mods.t], writes=[T_out])
    p.barrier()

    def ffn_arena():
        ar.reset()
        B = {}
        B["nT"] = [Buf(ar.alloc("nT%d" % i, [P, KC, TT], BF16), "nT%d" % i) for i in range(2)]
        hT_ap = ar.alloc("hT", [P, NFF, TT], BF16)
        B["hT_ap"] = hT_ap
        B["hT"] = [[Tl("hT%d_%d" % (c, h)) for h in range(2)] for c in range(NFF)]
        B["wd"] = [Buf(ar.alloc("wd%d" % i, [P, 11, D], BF16), "wd%d" % i) for i in range(2)]
        B["wr"] = [Buf(ar.alloc("wr%d" % i, [P, 2, KC, P], BF16), "wr%d" % i) for i in range(3)]
        B["xs"] = [Buf(ar.alloc("xs%d" % i, [P, D], F32), "xs%d" % i) for i in range(3)]
        B["xh"] = [Buf(ar.alloc("xh%d" % i, [P, D], BF16), "xh%d" % i) for i in range(2)]
        B["sg"] = [Buf(ar.alloc("sg%d" % i, [P, 512], BF16), "sg%d" % i) for i in range(2)]
        B["ho"] = [Buf(ar.alloc("ho%d" % i, [P, D], F32), "ho%d" % i) for i in range(2)]
        B["G"] = [Buf(ar.alloc("G%d" % i, [P, D], F32), "G%d" % i) for i in range(2)]
        B["junk"] = [Buf(ar.alloc("junk%d" % i, [P, D], BF16), "junk%d" % i) for i in range(2)]
        B["qks"] = [Buf(ar.alloc("qks%d" % i, [P, 512], BF16), "qks%d" % i) for i in range(3)]
        B["vst"] = [Buf(ar.alloc("vst%d" % i, [P, 4, 65], BF16), "vst%d" % i) for i in range(2)]
        B["vbst"] = [Buf(ar.alloc("vbst%d" % i, [P, 256], BF16), "vbst%d" % i) for i in range(2)]
        B["qis"] = [Buf(ar.alloc("qis%d" % i, [P, 512], F32), "qis%d" % i) for i in range(2)]
        B["qiTs"] = [Buf(ar.alloc("qiTs%d" % i, [P, 4, P], F32), "qiTs%d" % i) for i in range(2)]
        B["kis"] = [Buf(ar.alloc("kis%d" % i, [P, 64], F32), "kis%d" % i) for i in range(2)]
        B["kiTs"] = [Buf(ar.alloc("kiTs%d" % i, [64, P], F32), "kiTs%d" % i) for i in range(2)]
        B["wabs"] = [Buf(ar.alloc("wabs%d" % i, [P, 8], F32), "wabs%d" % i) for i in range(2)]
        B["sgn"] = [Buf(ar.alloc("sgn%d" % i, [P, 8], F32), "sgn%d" % i) for i in range(2)]
        B["wkw"] = Buf(ar.alloc("wkw", [P, KC, 72], BF16), "wkw")
        B["kwsb"] = [Buf(ar.alloc("kwsb%d" % i, [P, 72], F32), "kwsb%d" % i) for i in range(2)]
        B["cnt"] = {}
        return B

    ctr = {}

    def rot(key, lst):
        ctr[key] = ctr.get(key, -1) + 1
        return lst[ctr[key] % len(lst)]

    def rstd_from_ss(ss_ap, ss_t, inv_n, n=1):
        v = small()
        r = small()
        p.op("pool", lambda e: e.tensor_scalar(v.ap[:, 0:n], ss_ap, inv_n, EPS, ALU.mult, ALU.add),
             reads=[ss_t], writes=[v.t])
        p.op("pool", lambda e: e.tensor_tensor(r.ap[:, 0:n], v.ap[:, 0:n],
                                               neghalf.ap.to_broadcast([P, n]) if n > 1 else neghalf.ap,
                                               ALU.pow),
             reads=[v.t, neghalf.t], writes=[r.t])
        return r

    def prenorm_T(B, src, groups, acol, scol, dstT, col0, tbank):
        ng = len(groups)
        ss = small()
        for gi, (c0, c1) in enumerate(groups):
            jk = rot("junk", B["junk"])
            p.op("act", lambda e, gi=gi, c0=c0, c1=c1, jk=jk: e.activation(
                jk.ap[:, c0:c1], src.ap[:, c0:c1], AF.Square, accum_out=ss.ap[:, gi:gi + 1]),
                reads=[src.t], writes=[ss.t, jk.t])
        r = rstd_from_ss(ss.ap[:, 0:ng], ss.t, 1.0 / (groups[0][1] - groups[0][0]), ng)
        xh = rot("xh", B["xh"])
        for gi, (c0, c1) in enumerate(groups):
            p.op("dve", lambda e, gi=gi, c0=c0, c1=c1: e.tensor_scalar(
                xh.ap[:, c0:c1], src.ap[:, c0:c1], r.ap[:, gi:gi + 1], None, ALU.mult),
                reads=[src.t, r.t], writes=[xh.t])
        pst = pbf(tbank).rearrange("p (k c) -> p k c", k=KC)

        def fn_t(e):
            rr = None
            for k in range(KC):
                rr = e.transpose(pst[:, k, :], xh.ap[:, k * P:(k + 1) * P], identb.ap)
            return rr
        p.op("pe", fn_t, reads=[xh.t, identb.t], writes=[pb[tbank]])
        for k in range(KC):
            if scol is not None:
                p.op("dve", lambda e, k=k: e.tensor_scalar(
                    dstT.ap[:, k, col0:col0 + P], pst[:, k, :], acol[:, k:k + 1], scol[:, k:k + 1],
                    ALU.mult, ALU.add), reads=[pb[tbank], Acol.t, Scol.t], writes=[dstT.t])
            else:
                p.op("dve", lambda e, k=k: e.tensor_scalar(
                    dstT.ap[:, k, col0:col0 + P], pst[:, k, :], acol[:, k:k + 1], None, ALU.mult),
                    reads=[pb[tbank], goutT.t], writes=[dstT.t])

    def load_G(B, b, i):
        G = rot("G", B["G"])
        p.dma("sp", lambda e: e.dma_start(out=G.ap, in_=grow_s[b, i, :].partition_broadcast(P)), G.t,
              reads=[T_grow], writes=[G.t])
        return G

    def post_residual(B, yb, G, res_ap, res_tiles, dst_ap, dst_tile, nxt):
        yv = ps[:, yb:yb + 2, :].rearrange("p a c -> p (a c)")
        ss = small()
        jk = rot("junk", B["junk"])
        p.op("act", lambda e: e.activation(jk.ap, yv, AF.Square, accum_out=ss.ap[:, 0:1]),
             reads=[pb[yb], pb[yb + 1]], writes=[ss.t, jk.t])
        r = rstd_from_ss(ss.ap[:, 0:1], ss.t, 1.0 / D)
        xs = rot("xs", B["xs"])
        p.dma("sp", lambda e: e.dma_start(out=xs.ap, in_=res_ap), xs.t, reads=res_tiles, writes=[xs.t])
        ho = rot("ho", B["ho"])
        p.op("dve", lambda e: e.scalar_tensor_tensor(ho.ap, yv, r.ap[:, 0:1], G.ap, ALU.mult, ALU.mult),
             reads=[pb[yb], pb[yb + 1], r.t, G.t], writes=[ho.t])
        p.op("dve", lambda e: e.tensor_tensor(ho.ap, ho.ap, xs.ap, ALU.add), reads=[xs.t], writes=[ho.t])
        p.dma("sp", lambda e: e.dma_start(out=dst_ap, in_=ho.ap), ho.t, reads=[ho.t], writes=[dst_tile])
        if nxt is not None:
            acol, scol, dstT, col0, tbank = nxt
            prenorm_T(B, ho, [(0, D)], acol, scol, dstT, col0, tbank)

    def load_wd(B, f):
        for i in range(2):
            wd = B["wd"][i]
            p.dma("pool", lambda e, wd=wd, i=i: e.dma_start(out=wd.ap, in_=wd_d[f][:, i * 11:(i + 1) * 11, :]),
                  wd.t, writes=[wd.t])

    def load_wgu(B, f, ffc):
        wr = rot("wr", B["wr"])
        p.dma("pool", lambda e: e.dma_start(out=wr.ap.rearrange("p a k c -> p a (k c)"),
                                            in_=wgu_d[f][ffc].rearrange("p a k c -> p a (k c)")),
              wr.t, writes=[wr.t])
        return wr

    def gate_up(B, f, nT):
        load_wd(B, f)
        PF = 2
        slots = {}
        for c in range(min(PF, NFF)):
            slots[c] = load_wgu(B, f, c)
        for ffc in range(NFF):
            if ffc + PF < NFF:
                slots[ffc + PF] = load_wgu(B, f, ffc + PF)
            wr = slots.pop(ffc)
            for half in range(2):
                idx = ffc * 2 + half
                gb, ub = idx % 2, 2 + idx % 2
                cs = slice(half * 512, (half + 1) * 512)

                def fn_g(e, wr=wr, gb=gb, cs=cs, a=0):
                    r = None
                    for k in range(KC):
                        r = e.matmul(ps[:, gb, :], wr.ap[:, a, k, :], nT.ap[:, k, cs], start=(k == 0),
                                     stop=(k == KC - 1))
                    return r

                def fn_u(e, wr=wr, ub=ub, cs=cs):
                    r = None
                    for k in range(KC):
                        r = e.matmul(ps[:, ub, :], wr.ap[:, 1, k, :], nT.ap[:, k, cs], start=(k == 0),
                                     stop=(k == KC - 1))
                    return r
                p.op("pe", fn_g, reads=[wr.t, nT.t], writes=[pb[gb]])
                p.op("pe", fn_u, reads=[wr.t, nT.t], writes=[pb[ub]])
                sg = rot("sg", B["sg"])
                p.op("act", lambda e, sg=sg, gb=gb: e.activation(sg.ap, ps[:, gb, :], AF.Silu),
                     reads=[pb[gb]], writes=[sg.t])
                p.op("dve", lambda e, sg=sg, ub=ub, ffc=ffc, cs=cs: e.tensor_tensor(
                    B["hT_ap"][:, ffc, cs], sg.ap, ps[:, ub, :], ALU.mult),
                    reads=[sg.t, pb[ub]], writes=[B["hT"][ffc][half]])

    def down(B, s, yb):
        th = s // 4
        for half in range(2):
            def fn(e, half=half):
                r = None
                for c in range(NFF):
                    wd = B["wd"][c // 11]
                    r = e.matmul(ps[:, yb + half, :], B["hT_ap"][:, c, s * P:(s + 1) * P],
                                 wd.ap[:, c % 11, half * 512:(half + 1) * 512], start=(c == 0), stop=(c == NFF - 1))
                return r
            p.op("pe", fn, reads=[B["hT"][c][th] for c in range(NFF)] + [B["wd"][0].t, B["wd"][1].t],
                 writes=[pb[yb + half]])

    def project(B, b, ti, nT):
        t0 = ti * TT
        for c2 in range(8):
            wr = rot("wr", B["wr"])
            p.dma("pool", lambda e, wr=wr, c2=c2: e.dma_start(
                out=wr.ap.rearrange("p a k c -> p a (k c)"), in_=winfm_d[c2].rearrange("p a k c -> p a (k c)")),
                wr.t, writes=[wr.t])
            for a in range(2):
                ch = c2 * 2 + a
                kind, pair = ch // 4, ch % 4
                scale = 0.125 if kind in (0, 2) else 1.0
                for half in range(2):
                    bk = rot("pjb", [0, 1, 2, 3])
                    cs = slice(half * 512, (half + 1) * 512)

                    def fn(e, wr=wr, a=a, bk=bk, cs=cs):
                        r = None
                        for k in range(KC):
                            r = e.matmul(ps[:, bk, :], wr.ap[:, a, k, :], nT.ap[:, k, cs], start=(k == 0),
                                         stop=(k == KC - 1))
                        return r
                    p.op("pe", fn, reads=[wr.t, nT.t], writes=[pb[bk]])
                    st = rot("qks", B["qks"])
                    p.op("act", lambda e, st=st, bk=bk, scale=scale: e.activation(
                        st.ap, ps[:, bk, :], AF.Copy, scale=scale), reads=[pb[bk]], writes=[st.t])
                    p.dma("sp", lambda e, st=st, kind=kind, pair=pair, half=half: e.dma_start(
                        out=qkT_s[b, kind, pair, :, t0 + half * 512:t0 + (half + 1) * 512], in_=st.ap),
                        st.t, reads=[st.t], writes=[])
        for qv in range(4):
            wr = rot("wr", B["wr"])
            wv = wr.ap.rearrange("p a k c -> p (a k c)").rearrange("p (k c) -> p k c", k=KC)
            p.dma("pool", lambda e, wv=wv, wr=wr, qv=qv: e.dma_start(out=wv, in_=winv_d[qv]), wr.t,
                  writes=[wr.t])
            for s in range(NSUB):
                bk = rot("pjb", [0, 1, 2, 3])

                def fn(e, wv=wv, bk=bk, s=s):
                    r = None
                    for k in range(KC):
                        r = e.matmul(ps[:, bk, 0:256], nT.ap[:, k, s * P:(s + 1) * P], wv[:, k, :],
                                     start=(k == 0), stop=(k == KC - 1))
                    return r
                p.op("pe", fn, reads=[wr.t, nT.t], writes=[pb[bk]])
                rows = slice(t0 + s * P, t0 + (s + 1) * P)
                if qv < 2:
                    st = rot("vst", B["vst"])
                    p.op("act", lambda e, st=st, bk=bk: e.activation(
                        st.ap[:, :, 0:64], ps[:, bk, 0:256].rearrange("p (h d) -> p h d", h=4), AF.Copy),
                        reads=[pb[bk]], writes=[st.t])
                    p.dma("sp", lambda e, st=st, rows=rows, qv=qv: e.dma_start(
                        out=va_s[b, rows, qv * 4:(qv + 1) * 4, :], in_=st.ap), st.t, reads=[st.t],
                        writes=[])
                else:
                    st = rot("vbst", B["vbst"])
                    p.op("act", lambda e, st=st, bk=bk: e.activation(st.ap, ps[:, bk, 0:256], AF.Copy),
                         reads=[pb[bk]], writes=[st.t])
                    p.dma("sp", lambda e, st=st, rows=rows, qv=qv: e.dma_start(
                        out=vb_s[b, rows, (qv - 2) * 256:(qv - 1) * 256], in_=st.ap), st.t, reads=[st.t],
                        writes=[])
        if debug and "noidx" in debug.get("_variant", ""):
            return
        wq = []
        for hq in range(2):
            wr = rot("wr", B["wr"])
            wv = wr.ap.rearrange("p a k c -> p (a k c)").rearrange("p (k c) -> p k c", k=KC)
            p.dma("pool", lambda e, wv=wv, hq=hq: e.dma_start(out=wv, in_=winqi_d[hq]), wr.t, writes=[wr.t])
            wq.append((wr, wv))
        wkw = B["wkw"]
        p.dma("pool", lambda e: e.dma_start(out=wkw.ap, in_=winkw_d), wkw.t, writes=[wkw.t])
        for s in range(NSUB):
            rows = slice(t0 + s * P, t0 + (s + 1) * P)
            bq = rot("pjb", [0, 1, 2, 3])
            bkw = rot("pjb", [0, 1, 2, 3])

            def fn_q(e, bq=bq, s=s):
                r = None
                for hq in range(2):
                    for k in range(KC):
                        r = e.matmul(ps[:, bq, hq * 256:(hq + 1) * 256], nT.ap[:, k, s * P:(s + 1) * P],
                                     wq[hq][1][:, k, :], start=(k == 0), stop=(k == KC - 1))
                return r
            p.op("pe", fn_q, reads=[wq[0][0].t, wq[1][0].t, nT.t], writes=[pb[bq]])

            def fn_kw(e, bkw=bkw, s=s):
                r = None
                for k in range(KC):
                    r = e.matmul(ps[:, bkw, 0:72], nT.ap[:, k, s * P:(s + 1) * P], wkw.ap[:, k, :],
                                 start=(k == 0), stop=(k == KC - 1))
                return r
            p.op("pe", fn_kw, reads=[wkw.t, nT.t], writes=[pb[bkw]])
            kw = rot("kwsb", B["kwsb"])
            p.op("dve", lambda e, kw=kw, bkw=bkw: e.tensor_copy(kw.ap, ps[:, bkw, 0:72]), reads=[pb[bkw]], writes=[kw.t])
            wabs = rot("wabs", B["wabs"])
            p.op("act", lambda e, wabs=wabs, kw=kw: e.activation(
                wabs.ap, kw.ap[:, 64:72], AF.Abs, scale=512.0 ** -0.5),
                reads=[kw.t], writes=[wabs.t])
            qt_ = (t0 + s * P) // P
            sg_ap = sgn_all.ap[:, b, qt_, :]
            p.op("dve", lambda e, sg_ap=sg_ap, kw=kw: e.tensor_scalar(
                sg_ap, kw.ap[:, 64:72], 0.0, 0.5, ALU.is_gt, ALU.subtract), reads=[kw.t], writes=[sgn_all.t])
            p.op("dve", lambda e, sg_ap=sg_ap: e.tensor_scalar(sg_ap, sg_ap, 2.0, None, ALU.mult),
                 reads=[sgn_all.t], writes=[sgn_all.t])
            qis = rot("qis", B["qis"])
            p.op("dve", lambda e, qis=qis, bq=bq, wabs=wabs: e.tensor_tensor(
                qis.ap.rearrange("p (h d) -> p h d", h=8), ps[:, bq, :].rearrange("p (h d) -> p h d", h=8),
                wabs.ap.unsqueeze(2).to_broadcast([P, 8, 64]), ALU.mult),
                reads=[pb[bq], wabs.t], writes=[qis.t])
            ss = small()
            jk = rot("junk", B["junk"])
            p.op("act", lambda e, kw=kw, ss=ss, jk=jk: e.activation(
                jk.ap[:, 0:64], kw.ap[:, 0:64], AF.Square, accum_out=ss.ap[:, 0:1]),
                reads=[kw.t], writes=[ss.t, jk.t])
            r = rstd_from_ss(ss.ap[:, 0:1], ss.t, 1.0 / 64)
            kis = rot("kis", B["kis"])
            p.op("dve", lambda e, kis=kis, kw=kw, r=r: e.scalar_tensor_tensor(
                kis.ap, kw.ap[:, 0:64], r.ap[:, 0:1], gk.ap, ALU.mult, ALU.mult),
                reads=[kw.t, r.t, gk.t], writes=[kis.t])
            if debug and "notr" in debug.get("_variant", ""):
                continue
            bt = rot("pjb", [0, 1, 2, 3])

            def fn_tr(e, bt=bt, qis=qis, kis=kis):
                r2 = None
                for c in range(4):
                    r2 = e.transpose(ps[:, bt, c * P:(c + 1) * P], qis.ap[:, c * P:(c + 1) * P], identf.ap)
                return r2
            p.op("pe", fn_tr, reads=[qis.t, identf.t], writes=[pb[bt]])
            bt2 = rot("pjb", [0, 1, 2, 3])
            p.op("pe", lambda e, bt2=bt2, kis=kis: e.transpose(ps[0:64, bt2, 0:P], kis.ap, identf.ap),
                 reads=[kis.t, identf.t], writes=[pb[bt2]])
            qiTs = rot("qiTs", B["qiTs"])
            kiTs = rot("kiTs", B["kiTs"])
            p.op("act", lambda e, qiTs=qiTs, bt=bt: e.activation(
                qiTs.ap.rearrange("p c t -> p (c t)"), ps[:, bt, :], AF.Copy), reads=[pb[bt]], writes=[qiTs.t])
            p.op("act", lambda e, kiTs=kiTs, bt2=bt2: e.activation(kiTs.ap, ps[0:64, bt2, 0:P], AF.Copy),
                 reads=[pb[bt2]], writes=[kiTs.t])
            p.dma("sp", lambda e, qiTs=qiTs, rows=rows: e.dma_start(
                out=qiT_s[b, :, :, rows].rearrange("c p t -> p c t"), in_=qiTs.ap), qiTs.t, reads=[qiTs.t],
                writes=[])
            p.dma("sp", lambda e, kiTs=kiTs, rows=rows: e.dma_start(out=kiT_s[b, :, rows], in_=kiTs.ap),
                  kiTs.t, reads=[kiTs.t], writes=[])

    def attention_phase():
        ar.reset()
        kT = Buf(ar.alloc("kT", [P, 4, S], BF16), "kT")
        qT = Buf(ar.alloc("qT", [P, 4, S], BF16), "qT")
        vv = Buf(ar.alloc("vv", [P, 16, 520], BF16), "vv")
        qiT = Buf(ar.alloc("qiT", [P, 4, S], F32), "qiT")
        kiT = Buf(ar.alloc("kiT", [P, S], F32), "kiT")
        sgn = Buf(ar.alloc("sgnA", [P, 16, 8], F32), "sgnA")
        tbp = Buf(ar.alloc("tbp", [P, 2, 8, P], BF16), "tbp")
        trawf = Buf(ar.alloc("trawf", [P, 2, 8, P], F32), "trawf")
        sbmask = Buf(ar.alloc("sbmask", [P, 4, 512], BF16), "sbmask")
        Sb = [Buf(ar.alloc("Sb%d" % i, [P, S], F32), "Sb%d" % i) for i in range(2)]
        rl = [Buf(ar.alloc("rl%d" % i, [P, 512], F32), "rl%d" % i) for i in range(3)]
        NM = Buf(ar.alloc("NM", [P, S], BF16), "NM")
        cjunk = [Buf(ar.alloc("cjunk%d" % i, [P, S], BF16), "cjunk%d" % i) for i in range(2)]
        NMT = [Buf(ar.alloc("NMT%d" % i, [P, 16, P], BF16), "NMT%d" % i) for i in range(2)]
        PT = [Buf(ar.alloc("PT%d" % i, [P, 512], BF16), "PT%d" % i) for i in range(4)]
        oA = [Buf(ar.alloc("oA%d" % i, [P, 8, 64], F32), "oA%d" % i) for i in range(2)]
        steps = [Buf(ar.alloc("steps%d" % i, [P, NIT], F32), "steps%d" % i) for i in range(2)]
        mcol = Buf(ar.alloc("mcol", [P, 8], F32), "mcol")
        sqt = [Buf(ar.alloc("sqt%d" % i, [P, S], BF16), "sqt%d" % i) for i in range(2)]
        nrm = [Buf(ar.alloc("nrm%d" % i, [P, 8, 4], F32), "nrm%d" % i) for i in range(2)]
        eb = [Buf(ar.alloc("eb%d" % i, [P, 512], F32), "eb%d" % i) for i in range(4)]
        spb = [Buf(ar.alloc("spb%d" % i, [P, 512], F32), "spb%d" % i) for i in range(4)]
        Rb = [Buf(ar.alloc("Rb%d" % i, [P, 512], F32), "Rb%d" % i) for i in range(2)]
        aTb = [Buf(ar.alloc("aTb%d" % i, [P, 512], BF16), "aTb%d" % i) for i in range(2)]
        oB = Buf(ar.alloc("oB", [P, 4, 512], F32), "oB")
        poshalf = Buf(ar.alloc("poshalf", [P, 1], F32), "poshalf")
        thrb = [Buf(ar.alloc("thr%d" % i, [P, 1], F32), "thr%d" % i) for i in range(2)]
        if debug and debug.get("_report"):
            print("ATT arena used", ar.off, "of", ar.size)
        p.op("dve", lambda e: e.memset(poshalf.ap, 0.5), writes=[poshalf.t])
        for bf in NMT + PT + Sb + [NM] + rl:
            p.op("dve", lambda e, bf=bf: e.memset(bf.ap, 0.0), writes=[bf.t])

        ld(trawf, traw_d)
        ld(sbmask, sbmask_d)
        for h in range(8):
            p.op("dve", lambda e, h=h: e.tensor_scalar(tbp.ap[:, :, h, :], trawf.ap[:, :, h, :], b31.ap[:, h:h + 1],
                                                       None, ALU.subtract), reads=[trawf.t, b31.t], writes=[tbp.t])

        def ldr(buf, out_ap, src, eng="sp"):
            p.dma(eng, lambda e: e.dma_start(out=out_ap, in_=src), buf.t, writes=[buf.t])


        def load_dsa(b):
            ldr(kT, kT.ap, qkT_s[b, 1].rearrange("c p s -> p c s"))
            ldr(qT, qT.ap, qkT_s[b, 0].rearrange("c p s -> p c s"))
            ldr(vv, vv.ap, va_s[b].rearrange("(q p) h d -> p q (h d)", p=P))
            ldr(qiT, qiT.ap, qiT_s[b].rearrange("c p s -> p c s"))
            ldr(kiT, kiT.ap[0:64, :], kiT_s[b])
            ldr(kiT, kiT.ap[64:128, :], kiT_s[b])

        def norm_bound(which, src, pair):
            nr = nrm[which]
            sq = rot("sqt", sqt)
            p.op("dve", lambda e: e.tensor_tensor(sq.ap, src.ap[:, pair, :], src.ap[:, pair, :], ALU.mult),
                 reads=[src.t], writes=[sq.t])
            for half in range(2):
                h = pair * 2 + half
                for ch in range(4):
                    bk = rot("nb", [0, 1, 2, 3])
                    p.op("pe", lambda e, half=half, ch=ch, bk=bk: e.matmul(
                        ps[:, bk, :], selh.ap[:, half, :], sq.ap[:, ch * 512:(ch + 1) * 512], start=True, stop=True),
                        reads=[sq.t, selh.t], writes=[pb[bk]])
                    p.op("dve", lambda e, h=h, ch=ch, bk=bk: e.tensor_reduce(
                        nr.ap[:, h, ch:ch + 1], ps[:, bk, :], AX.X, ALU.max), reads=[pb[bk]], writes=[nr.t])

        def shift_cols():
            for which, src in enumerate((qT, kT)):
                for pair in range(4):
                    norm_bound(which, src, pair)
            n2q = small()
            n2k = small()
            p.op("dve", lambda e: e.tensor_reduce(n2q.ap, nrm[0].ap, AX.X, ALU.max), reads=[nrm[0].t], writes=[n2q.t])
            p.op("dve", lambda e: e.tensor_reduce(n2k.ap, nrm[1].ap, AX.X, ALU.max), reads=[nrm[1].t], writes=[n2k.t])
            m2 = small()
            p.op("dve", lambda e: e.tensor_tensor(m2.ap, n2q.ap, n2k.ap, ALU.mult), reads=[n2q.t, n2k.t], writes=[m2.t])
            mm_ = small()
            p.op("pool", lambda e: e.tensor_tensor(mm_.ap, m2.ap, poshalf.ap.to_broadcast([P, 8]), ALU.pow),
                 reads=[m2.t, poshalf.t], writes=[mm_.t])
            p.op("dve", lambda e: e.scalar_tensor_tensor(mcol.ap, mm_.ap, -1.01, b31.ap, ALU.mult, ALU.add),
                 reads=[mm_.t, b31.t], writes=[mcol.t])

        def idx_head(b, qi, Sq, h, N):
            pair, r0 = h // 2, 64 * (h % 2)
            nch = (N + 511) // 512
            for ch in range(nch):
                w = min(512, N - ch * 512)
                bk = rot("ib", [0, 1])
                p.op("pe", lambda e, ch=ch, w=w, bk=bk: e.matmul(
                    ps[:, bk, 0:w], qiT.ap[r0:r0 + 64, pair, qi * P:(qi + 1) * P],
                    kiT.ap[r0:r0 + 64, ch * 512:ch * 512 + w], start=True, stop=True),
                    reads=[qiT.t, kiT.t], writes=[pb[bk]])
                r = rot("rl", rl)
                p.op("act", lambda e, r=r, w=w, bk=bk: e.activation(r.ap[:, 0:w], ps[:, bk, 0:w], AF.Relu),
                     reads=[pb[bk]], writes=[r.t])
                cs = slice(ch * 512, ch * 512 + w)
                if h == 0:
                    p.op("dve", lambda e, r=r, w=w, cs=cs: e.tensor_scalar(
                        Sq.ap[:, cs], r.ap[:, 0:w], sgn_all.ap[:, b, qi, 0:1], None, ALU.mult),
                        reads=[r.t, sgn_all.t], writes=[Sq.t])
                else:
                    p.op("dve", lambda e, r=r, w=w, cs=cs: e.scalar_tensor_tensor(
                        Sq.ap[:, cs], r.ap[:, 0:w], sgn_all.ap[:, b, qi, h:h + 1], Sq.ap[:, cs], ALU.mult, ALU.add),
                        reads=[r.t, sgn_all.t], writes=[Sq.t])

        def bisect_iter(Sq, stp, lo, it, N):
            mid = small()
            cnt = small()
            dl = small()
            lo2 = small()
            p.op("dve", lambda e: e.tensor_tensor(mid.ap[:, 0:1], lo.ap[:, 0:1], stp.ap[:, it:it + 1], ALU.add),
                 reads=[lo.t, stp.t], writes=[mid.t])
            cj = rot("cjunk", cjunk)
            p.op("dve", lambda e: e.tensor_scalar(cj.ap[:, 0:N], Sq.ap[:, 0:N], mid.ap[:, 0:1], None, ALU.is_ge, ALU.add,
                                                  accum_out=cnt.ap[:, 0:1]), reads=[Sq.t, mid.t], writes=[cnt.t, cj.t])
            p.op("dve", lambda e: e.scalar_tensor_tensor(dl.ap[:, 0:1], cnt.ap[:, 0:1], TOPK - 0.5, stp.ap[:, it:it + 1],
                                                         ALU.is_ge, ALU.mult), reads=[cnt.t, stp.t], writes=[dl.t])
            p.op("dve", lambda e: e.tensor_tensor(lo2.ap[:, 0:1], lo.ap[:, 0:1], dl.ap[:, 0:1], ALU.add),
                 reads=[lo.t, dl.t], writes=[lo2.t])
            return lo2

        selst = {}

        def dsa_selA(b, qi):
            N = P * (qi + 1)
            if qi < 2:
                return
            Sq = Sb[qi % 2]
            for h in range(8):
                idx_head(b, qi, Sq, h, N)
            bm = small()
            p.op("dve", lambda e: e.tensor_reduce(bm.ap[:, 0:1], Sq.ap[:, 0:N], AX.X, ALU.max,
                                                  apply_absolute_value=True), reads=[Sq.t], writes=[bm.t])
            p.op("dve", lambda e: e.tensor_tensor(Sq.ap[:, N - P:N], Sq.ap[:, N - P:N], negdiag.ap, ALU.add),
                 reads=[negdiag.t], writes=[Sq.t])
            stp = steps[qi % 2]
            bm2 = small()
            p.op("dve", lambda e: e.tensor_scalar(bm2.ap[:, 0:1], bm.ap[:, 0:1], 2.0, 1e-30, ALU.mult, ALU.add),
                 reads=[bm.t], writes=[bm2.t])
            p.op("dve", lambda e: e.tensor_scalar(stp.ap, pow2.ap, bm2.ap[:, 0:1], None, ALU.mult),
                 reads=[pow2.t, bm2.t], writes=[stp.t])
            lo = small()
            p.op("dve", lambda e, lo=lo: e.tensor_scalar(lo.ap[:, 0:1], bm.ap[:, 0:1], -1.0, None, ALU.mult),
                 reads=[bm.t], writes=[lo.t])
            for it in range(NIT):
                lo = bisect_iter(Sq, stp, lo, it, N)
            thr = thrb[qi % 2]
            p.op("dve", lambda e, lo=lo: e.tensor_copy(thr.ap, lo.ap[:, 0:1]), reads=[lo.t], writes=[thr.t])

        def dsa_selB(qi, nmt):
            N = P * (qi + 1)
            if qi < 2:
                if qi == 1:
                    p.op("dve", lambda e: e.memset(nmt.ap[:, 0, :], 0.0), writes=[nmt.t])
                p.op("dve", lambda e: e.tensor_copy(nmt.ap[:, qi, :], nmtc.ap), reads=[nmtc.t], writes=[nmt.t])
                return
            Sq = Sb[qi % 2]
            thr = thrb[qi % 2]
            p.op("dve", lambda e: e.tensor_scalar(NM.ap[:, 0:N], Sq.ap[:, 0:N], thr.ap[:, 0:1], NEG, ALU.is_lt, ALU.mult),
                 reads=[Sq.t, thr.t], writes=[NM.t])
            nmps = ps[:, 2:4, :].rearrange("p a c -> p (a c)").bitcast(BF16).rearrange("p (k c) -> p k c", c=P)

            def fn_nt(e):
                r2 = None
                for blk in range(qi + 1):
                    r2 = e.transpose(nmps[:, blk, :], NM.ap[:, blk * P:(blk + 1) * P], identb.ap)
                return r2
            p.op("pe", fn_nt, reads=[NM.t, identb.t], writes=[pb[2], pb[3]])
            p.op("act", lambda e: e.activation(nmt.ap[:, 0:qi + 1, :], nmps[:, 0:qi + 1, :], AF.Copy),
                 reads=[pb[2], pb[3]], writes=[nmt.t])

        def dsa_head(qi, nmt, h):
            pair, r0 = h // 2, 64 * (h % 2)
            ob = 6 + h // 4
            oc = (h % 4) * 65
            for g0 in range(0, qi + 1, 4):
                nb = min(4, qi + 1 - g0)
                bk = rot("lb", [4, 5])

                def fn_l(e, g0=g0, nb=nb, bk=bk):
                    r2 = None
                    for j in range(nb):
                        blk = g0 + j
                        near = blk >= qi - 1
                        o_ = ps[:, bk, j * P:(j + 1) * P]
                        e.matmul(o_, kT.ap[r0:r0 + 64, pair, blk * P:(blk + 1) * P],
                                 qT.ap[r0:r0 + 64, pair, qi * P:(qi + 1) * P], start=True, stop=False)
                        r2 = e.matmul(o_, identb.ap, nmt.ap[:, blk, :], start=False, stop=not near)
                        if near:
                            r2 = e.matmul(o_, identb.ap, tbp.ap[:, qi - blk, h, :], start=False, stop=True)
                    return r2
                p.op("pe", fn_l, reads=[kT.t, qT.t, nmt.t, identb.t, tbp.t], writes=[pb[bk]])
                pt = rot("PT", PT)
                p.op("act", lambda e, pt=pt, nb=nb, bk=bk: e.activation(
                    pt.ap[:, 0:nb * P], ps[:, bk, 0:nb * P], AF.Exp, bias=mcol.ap[:, h:h + 1], scale=1.0),
                    reads=[pb[bk], mcol.t], writes=[pt.t])

                def fn_av(e, g0=g0, nb=nb, pt=pt):
                    r2 = None
                    for j in range(nb):
                        blk = g0 + j
                        r2 = e.matmul(ps[:, ob, oc:oc + 65], pt.ap[:, j * P:(j + 1) * P],
                                      vv.ap[:, blk, h * 65:(h + 1) * 65], start=(blk == 0), stop=(blk == qi))
                    return r2
                p.op("pe", fn_av, reads=[pt.t, vv.t], writes=[pb[ob]])

        def dsa_out(b, qi):
            o_ = rot("oA", oA)
            for hb, ob in enumerate((6, 7)):
                ov = ps[:, ob, 0:260].rearrange("p (h d) -> p h d", d=65)
                rc = small()
                p.op("dve", lambda e, rc=rc, ov=ov: e.reciprocal(rc.ap[:, 0:4], ov[:, :, 64]), reads=[pb[ob]], writes=[rc.t])
                p.op("dve", lambda e, rc=rc, ov=ov, hb=hb: e.tensor_tensor(
                    o_.ap[:, hb * 4:(hb + 1) * 4, :], ov[:, :, 0:64], rc.ap[:, 0:4].unsqueeze(2).to_broadcast([P, 4, 64]),
                    ALU.mult), reads=[pb[ob], rc.t], writes=[o_.t])
            p.dma("sp", lambda e: e.dma_start(out=o_s[b, qi * P:(qi + 1) * P, 0:512],
                                              in_=o_.ap.rearrange("p h d -> p (h d)")), o_.t, reads=[o_.t])

        vB = vv.ap[:, :, 0:512]

        def load_sb(b):
            ldr(kT, kT.ap, qkT_s[b, 3].rearrange("c p s -> p c s"))
            ldr(qT, qT.ap, qkT_s[b, 2].rearrange("c p s -> p c s"))
            ldr(vv, vB, vb_s[b].rearrange("(q p) d -> p q d", p=P))

        def sb_A(g, pair, half, j):
            rr = j - 4 * g
            r0 = 64 * half
            A = half
            kk = kT.ap[r0:r0 + 64, pair, j * P:(j + 1) * P]
            qq = qT.ap[r0:r0 + 64, pair, g * 512:(g + 1) * 512]

            def fn_a(e):
                r2 = e.matmul(ps[:, A, :], kk, qq, start=True, stop=(rr < 0))
                if rr >= 0:
                    r2 = e.matmul(ps[:, A, :], identb.ap, sbmask.ap[:, rr, :], start=False, stop=True)
                return r2
            p.op("pe", fn_a, reads=[kT.t, qT.t, identb.t, sbmask.t], writes=[pb[A]])
            ebuf, sp = eb[half * 2 + j % 2], spb[half * 2 + j % 2]
            p.op("act", lambda e: e.activation(ebuf.ap, ps[:, A, :], AF.Exp), reads=[pb[A]], writes=[ebuf.t])
            p.op("act", lambda e: e.activation(sp.ap, ebuf.ap, AF.Ln, bias=1.0, scale=1.0), reads=[ebuf.t], writes=[sp.t])

        def sb_B(g, pair, half, j):
            top = 4 * g + 3
            rr = j - 4 * g
            h = pair * 2 + half
            r0 = 64 * half
            Bk, O = 2 + half, 6 + half
            kk = kT.ap[r0:r0 + 64, pair, j * P:(j + 1) * P]
            qq = qT.ap[r0:r0 + 64, pair, g * 512:(g + 1) * 512]
            sp, R, aT = spb[half * 2 + j % 2], Rb[half], aTb[half]

            def fn_b(e):
                mms = [(kk, qq), (negtri.ap, sp.ap)]
                if j < top:
                    mms.append((negones.ap, R.ap))
                if rr >= 0:
                    mms.append((identb.ap, sbmask.ap[:, rr, :]))
                r2 = None
                for i, (l_, r_) in enumerate(mms):
                    r2 = e.matmul(ps[:, Bk, :], l_, r_, start=(i == 0), stop=(i == len(mms) - 1))
                return r2
            p.op("pe", fn_b, reads=[kT.t, qT.t, sp.t, R.t, negtri.t, negones.t, identb.t, sbmask.t], writes=[pb[Bk]])
            if j > 0:
                if j == top:
                    p.op("dve", lambda e: e.tensor_copy(R.ap, sp.ap), reads=[sp.t], writes=[R.t])
                else:
                    p.op("dve", lambda e: e.tensor_tensor(R.ap, R.ap, sp.ap, ALU.add), reads=[sp.t], writes=[R.t])
            p.op("act", lambda e: e.activation(aT.ap, ps[:, Bk, :], AF.Exp), reads=[pb[Bk]], writes=[aT.t])

            def fn_av2(e):
                r2 = None
                for tt in range(4):
                    if 4 * g + tt >= j:
                        r2 = e.matmul(ps[:, O, tt * 64:(tt + 1) * 64], aT.ap[:, tt * P:(tt + 1) * P],
                                      vB[:, j, h * 64:(h + 1) * 64], start=False, stop=False, skip_group_check=True)
                return r2
            p.op("pe", fn_av2, reads=[aT.t, vv.t], writes=[pb[O]])

        def sb_pair(g, pair):
            top = 4 * g + 3
            for half in range(2):
                p.op("pe", lambda e, half=half: e.matmul(ps[:, 6 + half, 0:256], zerosb.ap[:, 0:P], zerosb.ap[:, 0:256],
                                                         start=True, stop=True), reads=[zerosb.t], writes=[pb[6 + half]])
            for half in range(2):
                sb_A(g, pair, half, top)
            for j in range(top, -1, -1):
                if j - 1 >= 0:
                    for half in range(2):
                        sb_A(g, pair, half, j - 1)
                for half in range(2):
                    sb_B(g, pair, half, j)
            for half in range(2):
                h = pair * 2 + half
                p.op("act", lambda e, half=half, h=h: e.activation(
                    oB.ap[:, :, h * 64:(h + 1) * 64], ps[:, 6 + half, 0:256].rearrange("p (t d) -> p t d", d=64), AF.Copy),
                    reads=[pb[6 + half]], writes=[oB.t])

        def sb_group(b, g):
            for pair in range(4):
                sb_pair(g, pair)
            p.dma("sp", lambda e: e.dma_start(
                out=o_s[b, g * 512:(g + 1) * 512, 512:1024].rearrange("(t p) d -> p t d", p=P), in_=oB.ap),
                oB.t, reads=[oB.t])

        for b in range(NBC if not (debug and debug.get("_b1")) else 1):
            load_dsa(b)
            shift_cols()
            dsa_selA(b, 0)
            dsa_selB(0, NMT[0])
            for qi in range(16):
                if qi + 1 < 16:
                    dsa_selA(b, qi + 1)
                for h in range(8):
                    dsa_head(qi, NMT[qi % 2], h)
                dsa_out(b, qi)
                if qi + 1 < 16:
                    dsa_selB(qi + 1, NMT[(qi + 1) % 2])
            load_sb(b)
            for g in range(4):
                sb_group(b, g)

    def phase3():
        B = ffn_arena()
        wout_ap = B["wd"][1].ap[:, 0:8, :]
        wout_t = B["wd"][1].t
        p.op("dve", lambda e: e.memset(B["wd"][1].ap, 0.0), writes=[wout_t])
        tiles3 = [(b, ti) for b in range(NBC) for ti in range(NTILE)]
        if debug and debug.get("_b1"):
            tiles3 = tiles3[:NTILE]

        def pro3(b, ti, s, dstT):
            xs = rot("xs", B["xs"])
            rows = slice(ti * TT + s * P, ti * TT + (s + 1) * P)
            p.dma("sp", lambda e: e.dma_start(out=xs.ap, in_=o_s[b, rows, :]), xs.t, writes=[xs.t])
            prenorm_T(B, xs, [(0, 512), (512, D)], goutT.ap, None, dstT, s * P, 0)

        onT = rot("nT", B["nT"])
        for s in range(NSUB):
            pro3(tiles3[0][0], tiles3[0][1], s, onT)
        for idx, (b, ti) in enumerate(tiles3):
            G2 = load_G(B, b, 1)
            G3 = load_G(B, b, 2)
            p.dma("pool", lambda e: e.dma_start(out=wout_ap, in_=wout_d), wout_t, writes=[wout_t])
            n3T = rot("nT", B["nT"])
            pend = None
            for s in range(NSUB):
                yb = 4 + 2 * (s % 2)
                for half in range(2):
                    def fn(e, half=half, s=s, yb=yb, onT=onT):
                        r = None
                        for k in range(KC):
                            r = e.matmul(ps[:, yb + half, :], onT.ap[:, k, s * P:(s + 1) * P],
                                         wout_ap[:, k, half * 512:(half + 1) * 512], start=(k == 0), stop=(k == KC - 1))
                        return r
                    p.op("pe", fn, reads=[onT.t, wout_t], writes=[pb[yb + half]])
                if pend is not None:
                    pend()
                r0 = ti * TT + s * P
                rows = slice(r0, r0 + P)

                def epi(s=s, yb=yb, rows=rows, r0=r0):
                    post_residual(B, yb, G2, h1_s[b, rows, :], [], h2_s[b, rows, :], T_h2[b][r0 // P],
                                  (Acol.ap[:, 2, b, :], Scol.ap[:, 2, b, :], n3T, s * P, 1))
                pend = epi
            pend()
            gate_up(B, 1, n3T)
            nxt = tiles3[idx + 1] if idx + 1 < len(tiles3) else None
            if nxt is not None:
                onT = rot("nT", B["nT"])
            pend = None
            for s in range(NSUB):
                yb = 4 + 2 * (s % 2)
                down(B, s, yb)
                if nxt is not None:
                    pro3(nxt[0], nxt[1], s, onT)
                if pend is not None:
                    pend()
                r0 = ti * TT + s * P
                rows = slice(r0, r0 + P)

                def epi2(s=s, yb=yb, rows=rows, r0=r0):
                    post_residual(B, yb, G3, h2_s[b, rows, :], [T_h2[b][r0 // P]], out_d[b, rows, :], Tl("o"), None)
                pend = epi2
            pend()

    B = ffn_arena()
    for st in B["vst"]:
        p.op("dve", lambda e, st=st: e.memset(st.ap, 1.0), writes=[st.t])

    def prologue_sub(B, b, ti, s, dstT):
        xs = rot("xs", B["xs"])
        rows = slice(ti * TT + s * P, ti * TT + (s + 1) * P)
        p.dma("sp", lambda e: e.dma_start(out=xs.ap, in_=x_d[b, rows, :]), xs.t, writes=[xs.t])
        prenorm_T(B, xs, [(0, D)], Acol.ap[:, 0, b, :], Scol.ap[:, 0, b, :], dstT, s * P, 0)

    tiles = [(b, ti) for b in range(NBC) for ti in range(NTILE)]
    if debug and "_ntiles" in debug:
        tiles = tiles[:debug["_ntiles"]]
    if debug and "_tiles" in debug:
        tiles = debug["_tiles"]
    nT_cur = rot("nT", B["nT"])
    for s in range(NSUB):
        prologue_sub(B, tiles[0][0], tiles[0][1], s, nT_cur)
    for idx, (b, ti) in enumerate(tiles):
        G = load_G(B, b, 0)
        gate_up(B, 0, nT_cur)
        n2T = rot("nT", B["nT"])
        nxt = tiles[idx + 1] if idx + 1 < len(tiles) else None
        pend = None
        if nxt is not None:
            nT_cur = rot("nT", B["nT"])
        for s in range(NSUB):
            yb = 4 + 2 * (s % 2)
            down(B, s, yb)
            if nxt is not None:
                prologue_sub(B, nxt[0], nxt[1], s, nT_cur)
            if pend is not None:
                pend()
            r0 = ti * TT + s * P
            rows = slice(r0, r0 + P)

            def epi(s=s, yb=yb, rows=rows, r0=r0):
                post_residual(B, yb, G, x_d[b, rows, :], [], h1_s[b, rows, :], Tl('h1'),
                              (Acol.ap[:, 1, b, :], Scol.ap[:, 1, b, :], n2T, s * P, 1))
            pend = epi
        pend()
        if not (debug and "noproj" in debug.get("_variant", "")):
            project(B, b, ti, n2T)
    p.barrier()


    stop_at = debug.get("_stop", 99) if debug else 99
    if stop_at >= 2:
        attention_phase()
        p.barrier()
    if stop_at >= 3:
        phase3()
        p.barrier()

    p.barrier()
    with nc.Block() as block:
        @block.tensor
        def _(e):
            p.run(e, "pe")

        @block.scalar
        def _(e):
            p.run(e, "act")

        @block.vector
        def _(e):
            p.run(e, "dve")

        @block.gpsimd
        def _(e):
            p.run(e, "pool")

        @block.sync
        def _(e):
            p.run(e, "sp")
    return nc, stack, p


def _t5_bucket_np(n):
    n = np.maximum(n, 0)
    max_exact = 16
    nf = np.maximum(n, 1).astype(np.float32)
    large = max_exact + (np.log(nf / max_exact) / np.log(128 / max_exact) * (32 - max_exact)).astype(np.int32)
    large = np.minimum(large, 31)
    return np.where(n < max_exact, n, large)


def host_consts():
    bf = ml_dtypes.bfloat16
    c = {}
    c["identb"] = np.eye(P, dtype=np.float32).astype(bf)
    c["identf"] = np.eye(P, dtype=np.float32)
    jj = np.arange(P)[:, None]
    ss = np.arange(P)[None, :]
    c["negtri"] = np.where(jj >= ss, -1.0, 0.0).astype(np.float32)
    c["nmtc"] = np.where(jj > ss, NEG, 0.0).astype(np.float32).astype(bf)
    c["negdiag"] = np.where(ss > jj, -1e30, 0.0).astype(np.float32)
    t512 = np.arange(512)[None, None, :]
    r4 = np.arange(4)[None, :, None]
    s128 = np.arange(P)[:, None, None]
    c["sbmask"] = np.where(r4 * P + s128 < t512, 0.0, NEG).astype(np.float32).astype(bf)
    c["pow2"] = np.broadcast_to((2.0 ** -(np.arange(NIT) + 1.0)).astype(np.float32)[None, :], (P, NIT)).copy()
    selb = np.zeros((2, 2, P), np.float32)
    selb[0, 0, :] = 1.0
    selb[1, 1, :] = 1.0
    c["selb"] = selb
    selh = np.zeros((P, 2, P), np.float32)
    selh[:64, 0, :] = 1.0
    selh[64:, 1, :] = 1.0
    c["selh"] = selh.astype(bf)
    return c


def host_shared(inp):
    f = np.float32
    sh = {}
    w_ada = np.asarray(inp["w_ada"], f)[0]
    sh["wada"] = np.ascontiguousarray(w_ada.reshape(KC, P, 18, 512).transpose(2, 1, 0, 3))
    sh["bada"] = np.ascontiguousarray(np.asarray(inp["b_ada"], f)[0])
    sh["gpreT"] = np.ascontiguousarray(np.asarray(inp["g_pre"], f)[0].reshape(3, KC, P).transpose(2, 0, 1))
    sh["gpost"] = np.ascontiguousarray(np.asarray(inp["g_post"], f)[0])
    for ff in range(2):
        wg = np.asarray(inp["w_ffn_gate"], f)[0, ff].reshape(KC, P, NFF, P)
        wu = np.asarray(inp["w_ffn_up"], f)[0, ff].reshape(KC, P, NFF, P)
        gu = np.stack([wg, wu], axis=0)
        sh["wgu%d" % ff] = np.ascontiguousarray(gu.transpose(3, 2, 0, 1, 4))
        wd = np.asarray(inp["w_ffn_down"], f)[0, ff].reshape(NFF, P, D)
        sh["wd%d" % ff] = np.ascontiguousarray(wd.transpose(1, 0, 2))
    w_in = np.asarray(inp["w_in"], f)[0]
    qa, ka, va = w_in[:, 0:512], w_in[:, 512:1024], w_in[:, 1024:1536]
    qi, kiw = w_in[:, 1536:2048], w_in[:, 2048:2120]
    qb, kb, vb = w_in[:, 2120:2632], w_in[:, 2632:3144], w_in[:, 3144:3656]
    fm = np.concatenate([qa, ka, qb, kb], axis=1).reshape(KC, P, 8, 2, P)
    sh["winfm"] = np.ascontiguousarray(fm.transpose(2, 1, 3, 0, 4))
    vv = np.concatenate([va, vb], axis=1).reshape(KC, P, 4, 256)
    sh["winv"] = np.ascontiguousarray(vv.transpose(2, 1, 0, 3))
    sh["winqi"] = np.ascontiguousarray(qi.reshape(KC, P, 2, 256).transpose(2, 1, 0, 3))
    sh["winkw"] = np.ascontiguousarray(kiw.reshape(KC, P, 72).transpose(1, 0, 2))
    sh["wout"] = np.ascontiguousarray(np.asarray(inp["w_out"], f)[0].reshape(KC, P, D).transpose(1, 0, 2))
    sh["gk"] = np.ascontiguousarray(np.broadcast_to(np.asarray(inp["g_kidx"], f)[0][None, :], (P, 64)))
    gout = np.concatenate([np.asarray(inp["g_out_a"], f)[0], np.asarray(inp["g_out_b"], f)[0]])
    sh["goutT"] = np.ascontiguousarray(gout.reshape(KC, P).T)
    rb = np.asarray(inp["rel_bias"], f)
    s_ = np.arange(P)[:, None]
    t_ = np.arange(P)[None, :]
    tr = np.zeros((P, 2, 8, P), f)
    for v in range(2):
        bk = _t5_bucket_np(t_ - s_ + v * P)
        tr[:, v, :, :] = rb[bk].transpose(0, 2, 1)
    sh["traw"] = tr
    sh["b31"] = np.ascontiguousarray(np.broadcast_to(rb[31][None, :], (P, 8)))
    sh.update(host_consts())
    return sh


_CACHE = {}


def kernel(**inputs):
    x = np.asarray(inputs["x"], np.float32)
    c = np.asarray(inputs["c"], np.float32)
    sh = host_shared(inputs)
    if "nc" not in _CACHE:
        _CACHE["nc"] = build_program()
    nc, stack, prog = _CACHE["nc"]
    in_maps = []
    for i in range(N_CORES):
        m = dict(sh)
        m["x"] = np.ascontiguousarray(x[NBC * i:NBC * (i + 1)])
        m["cT"] = np.ascontiguousarray(c[NBC * i:NBC * (i + 1)].reshape(NBC, KC, P).transpose(2, 1, 0))
        in_maps.append(m)
    res = run_bass_kernel_spmd(nc, in_maps, core_ids=list(range(N_CORES)))
    out = np.concatenate([np.asarray(r["out"]) for r in res.results], axis=0)
    return out.astype(np.float32)
```

```python
import numpy as np
import ml_dtypes
from contextlib import ExitStack
import concourse.bass as bass
import concourse.mybir as mybir
from concourse.bass_utils import run_bass_kernel_spmd

F32 = mybir.dt.float32
BF16 = mybir.dt.bfloat16
AF = mybir.ActivationFunctionType
ALU = mybir.AluOpType
AX = mybir.AxisListType

P = 128
D = 1024
S = 2048
NBC = 2
DFF = 2816
NFF = 22
KC = 8
TT = 1024
NSUB = TT // P
NTILE = S // TT
EPS = 1e-6
NIT = 18
TOPK = 256
NEG = -30000.0
WIN_COLS = 3656
N_CORES = 8


class Tl:
    __slots__ = ("name", "w", "r", "dsem", "dcnt")

    def __init__(self, name):
        self.name = name
        self.w = None
        self.r = {}
        self.dsem = None
        self.dcnt = 0


class Prog:
    ENG = ("pe", "act", "dve", "pool", "sp")

    def __init__(self, nc, stack):
        self.nc = nc
        self.stack = stack
        self.sems = []
        self.esid = {}
        for e in self.ENG:
            self.esid[e] = self.newsem("s_" + e)
        self.cnt = {e: 0 for e in self.ENG}
        self.q = {e: [] for e in self.ENG}
        self.waited = {e: {} for e in self.ENG}
        self.dtiles = []
        self.nops = 0

    def newsem(self, name):
        name = "%s_%d" % (name, len(self.sems))
        h = self.stack.enter_context(self.nc.semaphore(name))
        self.sems.append(h)
        return len(self.sems) - 1

    @staticmethod
    def _add(d, sv):
        if sv is None:
            return
        s, v = sv
        if d.get(s, 0) < v:
            d[s] = v

    def _collect(self, reads, writes):
        d = {}
        for t in reads:
            self._add(d, t.w)
        for t in writes:
            self._add(d, t.w)
            for s, v in t.r.items():
                self._add(d, (s, v))
        return d

    def _emit(self, eng, d, fn, sid, inc):
        w = self.waited[eng]
        waits = []
        for s, v in d.items():
            if w.get(s, 0) < v:
                w[s] = v
                waits.append((s, v))
        self.q[eng].append((waits, fn, sid, inc))
        self.nops += 1

    def _mark(self, tk, reads, writes):
        for t in writes:
            t.w = tk
            t.r = {}
        for t in reads:
            if t not in writes:
                if t.r.get(tk[0], 0) < tk[1]:
                    t.r[tk[0]] = tk[1]

    def op(self, eng, fn, reads=(), writes=()):
        d = self._collect(reads, writes)
        self.cnt[eng] += 1
        tk = (self.esid[eng], self.cnt[eng])
        self._emit(eng, d, fn, tk[0], 1)
        self._mark(tk, reads, writes)
        return tk

    def dma(self, eng, fn, semtile, reads=(), writes=()):
        d = self._collect(reads, writes)
        if semtile.dsem is None:
            semtile.dsem = self.newsem("d_" + semtile.name)
            self.dtiles.append(semtile)
        if semtile.dcnt > 0:
            self._add(d, (semtile.dsem, semtile.dcnt))
        semtile.dcnt += 16
        tk = (semtile.dsem, semtile.dcnt)
        self._emit(eng, d, fn, tk[0], 16)
        self._mark(tk, reads, writes)
        return tk

    def barrier(self):
        d = {}
        for e in self.ENG:
            if self.cnt[e] > 0:
                d[self.esid[e]] = self.cnt[e]
        for t in self.dtiles:
            d[t.dsem] = t.dcnt
        for e in self.ENG:
            self._emit(e, dict(d), None, None, 0)

    def run(self, e, name):
        sems = self.sems
        for waits, fn, sid, inc in self.q[name]:
            for s, v in waits:
                e.wait_ge(sems[s], v)
            if fn is not None:
                ins = fn(e)
                ins.then_inc(sems[sid], inc)


_DBGNAMES = []


class Arena:
    def __init__(self, nc, base, size):
        self.nc = nc
        self.base = base
        self.size = size
        self.off = 0
        self.uid = 0
        self.peak = 0

    def reset(self):
        self.off = 0

    def alloc(self, name, shape, dtype):
        esz = 2 if dtype == BF16 else 4
        n = esz
        for s in shape[1:]:
            n *= s
        addr = (self.off + 63) // 64 * 64
        self.off = addr + n
        self.peak = max(self.peak, self.off)
        assert self.off <= self.size, (name, self.off, self.size)
        self.uid += 1
        h = self.nc.alloc_sbuf_tensor_at("%s_%d" % (name, self.uid), list(shape), dtype,
                                         offset=self.base + addr)
        if name in ("mcol", "Sb0", "Sb1", "NM", "NMT0", "NMT1", "steps0", "steps1") or name.startswith("small"):
            _DBGNAMES.append(h.name)
        return h.ap()


class Buf:
    __slots__ = ("ap", "t")

    def __init__(self, ap, name):
        self.ap = ap
        self.t = Tl(name)


def build_program(debug=None):
    nc = bass.Bass("TRN2", target_bir_lowering=False)
    stack = ExitStack()
    p = Prog(nc, stack)

    def din(name, shape, dt=F32):
        return nc.dram_tensor(name, list(shape), dt, kind="ExternalInput").ap()

    def dint(name, shape, dt=F32):
        kind = "ExternalOutput" if (debug and debug.get("_expose")) else "Internal"
        return nc.dram_tensor(name, list(shape), dt, kind=kind).ap()

    x_d = din("x", [NBC, S, D])
    cT_d = din("cT", [P, KC, NBC])
    wada_d = din("wada", [18, P, KC, 512])
    bada_d = din("bada", [9 * D])
    gpreT_d = din("gpreT", [P, 3, KC])
    gpost_d = din("gpost", [3, D])
    wgu_d = [din("wgu%d" % f, [NFF, P, 2, KC, P]) for f in range(2)]
    wd_d = [din("wd%d" % f, [P, NFF, D]) for f in range(2)]
    winfm_d = din("winfm", [8, P, 2, KC, P])
    winv_d = din("winv", [4, P, KC, 256])
    winqi_d = din("winqi", [2, P, KC, 256])
    winkw_d = din("winkw", [P, KC, 72])
    wout_d = din("wout", [P, KC, D])
    gk_d = din("gk", [P, 64])
    goutT_d = din("goutT", [P, KC])
    traw_d = din("traw", [P, 2, 8, P])
    b31_d = din("b31", [P, 8])
    identb_d = din("identb", [P, P], BF16)
    identf_d = din("identf", [P, P])
    negtri_d = din("negtri", [P, P])
    nmtc_d = din("nmtc", [P, P], BF16)
    negdiag_d = din("negdiag", [P, P])
    sbmask_d = din("sbmask", [P, 4, 512], BF16)
    pow2_d = din("pow2", [P, NIT])
    selb_d = din("selb", [2, 2, P])
    selh_d = din("selh", [P, 2, P], BF16)
    out_d = nc.dram_tensor("out", [NBC, S, D], F32, kind="ExternalOutput").ap()

    h1_s = dint("h1_s", [NBC, S, D])
    h2_s = dint("h2_s", [NBC, S, D])
    o_s = dint("o_s", [NBC, S, D])
    grow_s = dint("grow_s", [NBC, 3, D])
    qkT_s = dint("qkT_s", [NBC, 4, 4, P, S], BF16)
    va_s = dint("va_s", [NBC, S, 8, 65], BF16)
    vb_s = dint("vb_s", [NBC, S, 512], BF16)
    qiT_s = dint("qiT_s", [NBC, 4, P, S])
    kiT_s = dint("kiT_s", [NBC, 64, S])
    sgn_s = dint("sgn_s", [NBC, S, 8])

    dbg = {}
    if debug:
        for name, shape in debug.items():
            if name.startswith("_"):
                continue
            dbg[name] = nc.dram_tensor("dbg_" + name, list(shape), F32, kind="ExternalOutput").ap()

    T_h1 = [[Tl("h1_%d_%d" % (b, r)) for r in range(S // P)] for b in range(NBC)]
    T_h2 = [[Tl("h2_%d_%d" % (b, r)) for r in range(S // P)] for b in range(NBC)]
    T_o = [[Tl("o_%d_%d" % (b, r)) for r in range(S // P)] for b in range(NBC)]
    T_att = [Tl("att_%d" % b) for b in range(NBC)]
    T_grow = Tl("grow")
    T_out = Tl("out")

    arena_bytes = 204 * 1024
    arena_h = nc.alloc_sbuf_tensor("arena", [P, arena_bytes // 4], F32)
    base = nc.lookup_mloc(arena_h).addr
    pers = Arena(nc, base, 10 * 1024)
    ar = Arena(nc, base + 10 * 1024, arena_bytes - 10 * 1024)

    ps_h = nc.alloc_psum_tensor("ps", [P, 8, 512], F32)
    ps = ps_h.ap()
    pb = [Tl("pb%d" % i) for i in range(8)]

    def pbf(i):
        return ps[:, i, :].bitcast(BF16)

    identb = Buf(pers.alloc("identb", [P, P], BF16), "identb")
    identf = Buf(pers.alloc("identf", [P, P], F32), "identf")
    zerosb = Buf(pers.alloc("zerosb", [P, 512], BF16), "zerosb")
    negtri = Buf(pers.alloc("negtri", [P, P], F32), "negtri")
    negones = Buf(pers.alloc("negones", [P, P], F32), "negones")
    nmtc = Buf(pers.alloc("nmtc", [P, P], BF16), "nmtc")
    negdiag = Buf(pers.alloc("negdiag", [P, P], F32), "negdiag")
    pow2 = Buf(pers.alloc("pow2", [P, NIT], F32), "pow2")
    selh = Buf(pers.alloc("selh", [P, 2, P], BF16), "selh")
    gk = Buf(pers.alloc("gk", [P, 64], F32), "gk")
    goutT = Buf(pers.alloc("goutT", [P, KC], F32), "goutT")
    b31 = Buf(pers.alloc("b31", [P, 8], F32), "b31")
    Acol = Buf(pers.alloc("Acol", [P, 3, NBC, KC], F32), "Acol")
    Scol = Buf(pers.alloc("Scol", [P, 3, NBC, KC], F32), "Scol")
    neghalf = Buf(pers.alloc("neghalf", [P, 1], F32), "neghalf")
    sgn_all = Buf(pers.alloc("sgn_all", [P, NBC, 16, 8], F32), "sgn_all")
    smallr = [Buf(pers.alloc("small%d" % i, [P, 8], F32), "small%d" % i) for i in range(32)]
    small_i = [0]

    def small():
        small_i[0] += 1
        return smallr[small_i[0] % len(smallr)]

    def ld(buf, src, eng="sp"):
        p.dma(eng, lambda e, o=buf.ap, i=src: e.dma_start(out=o, in_=i), buf.t, writes=[buf.t])

    for bf, src in ((identb, identb_d), (identf, identf_d), (negtri, negtri_d), (nmtc, nmtc_d),
                    (negdiag, negdiag_d), (pow2, pow2_d), (selh, selh_d), (gk, gk_d),
                    (goutT, goutT_d), (b31, b31_d)):
        ld(bf, src)
    p.op("dve", lambda e: e.memset(zerosb.ap, 0.0), writes=[zerosb.t])
    p.op("dve", lambda e: e.memset(negones.ap, -1.0), writes=[negones.t])
    p.op("dve", lambda e: e.memset(neghalf.ap, -0.5), writes=[neghalf.t])

    ar.reset()
    cT = Buf(ar.alloc("cT", [P, KC, NBC], F32), "cT")
    siluT = Buf(ar.alloc("siluT", [P, KC, NBC], F32), "siluT")
    wada = [Buf(ar.alloc("wada%d" % i, [P, KC, 512], F32), "wada%d" % i) for i in range(2)]
    mods = Buf(ar.alloc("mods", [2, 9 * D], F32), "mods")
    bada2 = Buf(ar.alloc("bada2", [2, 9 * D], F32), "bada2")
    gpost2 = Buf(ar.alloc("gpost2", [2, 3, D], F32), "gpost2")
    grow = Buf(ar.alloc("grow", [2, 3, D], F32), "grow")
    selb = Buf(ar.alloc("selb", [2, 2, P], F32), "selb")
    gpreT = Buf(ar.alloc("gpreT", [P, 3, KC], F32), "gpreT")
    modT = Buf(ar.alloc("modT", [P, 48, 2], F32), "modT")

    ld(cT, cT_d)
    ld(gpreT, gpreT_d)
    ld(selb, selb_d)
    p.dma("sp", lambda e: e.dma_start(out=bada2.ap, in_=bada_d.partition_broadcast(2)), bada2.t,
          writes=[bada2.t])
    p.dma("sp", lambda e: e.dma_start(out=gpost2.ap.rearrange("p a d -> p (a d)"),
                                      in_=gpost_d.rearrange("a d -> (a d)").partition_broadcast(2)),
          gpost2.t, writes=[gpost2.t])
    p.op("act", lambda e: e.activation(siluT.ap, cT.ap, AF.Silu), reads=[cT.t], writes=[siluT.t])
    for g in range(18):
        wb = wada[g % 2]
        ld(wb, wada_d[g])
        bk = g % 2

        def fn(e, wb=wb, bk=bk):
            r = None
            for k in range(KC):
                r = e.matmul(ps[0:2, bk, :], siluT.ap[:, k, :], wb.ap[:, k, :], start=(k == 0), stop=(k == KC - 1))
            return r
        p.op("pe", fn, reads=[wb.t, siluT.t], writes=[pb[bk]])
        p.op("dve", lambda e, g=g, bk=bk: e.tensor_tensor(mods.ap[:, g * 512:(g + 1) * 512], ps[0:2, bk, :],
                                                           bada2.ap[:, g * 512:(g + 1) * 512], ALU.add),
             reads=[pb[bk], bada2.t], writes=[mods.t])
    modT_ps = ps[:, 2, 0:96].rearrange("p (c b) -> p c b", b=2)

    def fn_modT(e):
        r = None
        for i in range(3):
            for which in range(2):
                for k in range(KC):
                    off = (3 * i + which) * D + k * P
                    idx = (i * 2 + which) * KC + k
                    r = e.matmul(modT_ps[:, idx, :], mods.ap[0:2, off:off + P], identf.ap[0:2, 0:2],
                                 start=True, stop=True)
        return r
    p.op("pe", fn_modT, reads=[mods.t, identf.t], writes=[pb[2]])
    p.op("dve", lambda e: e.tensor_copy(modT.ap, modT_ps), reads=[pb[2]], writes=[modT.t])
    modT_v = modT.ap.rearrange("p (i w k) b -> p i w k b", i=3, w=2)
    for i in range(3):
        for b in range(NBC):
            p.op("dve", lambda e, i=i, b=b: e.tensor_copy(Scol.ap[:, i, b, :], modT_v[:, i, 0, :, b]),
                 reads=[modT.t], writes=[Scol.t])
            p.op("dve", lambda e, i=i, b=b: e.scalar_tensor_tensor(
                Acol.ap[:, i, b, :], modT_v[:, i, 1, :, b], 1.0, gpreT.ap[:, i, :], ALU.add, ALU.mult),
                reads=[modT.t, gpreT.t], writes=[Acol.t])
    for i in range(3):
        coef = 1.0 if i == 1 else 0.5
        p.op("dve", lambda e, i=i, coef=coef: e.scalar_tensor_tensor(
            grow.ap[:, i, :], mods.ap[:, (3 * i + 2) * D:(3 * i + 3) * D], coef, gpost2.ap[:, i, :],
            ALU.mult, ALU.mult), reads=[mods.t, gpost2.t], writes=[grow.t])
    p.dma("sp", lambda e: e.dma_start(out=grow_s, in_=grow.ap), grow.t, reads=[grow.t], writes=[T_grow])
    if "mods" in dbg:
        p.dma("sp", lambda e: e.dma_start(out=dbg["mods"], in_=mods.ap), mods.t, reads=[mods.t], writes=[T_out])
    p.barrier()

    def ffn_arena():
        ar.reset()
        B = {}
        B["nT"] = [Buf(ar.alloc("nT%d" % i, [P, KC, TT], BF16), "nT%d" % i) for i in range(2)]
        hT_ap = ar.alloc("hT", [P, NFF, TT], BF16)
        B["hT_ap"] = hT_ap
        B["hT"] = [[Tl("hT%d_%d" % (c, h)) for h in range(2)] for c in range(NFF)]
        B["wd"] = [Buf(ar.alloc("wd%d" % i, [P, 11, D], BF16), "wd%d" % i) for i in range(2)]
        B["wr"] = [Buf(ar.alloc("wr%d" % i, [P, 2, KC, P], BF16), "wr%d" % i) for i in range(3)]
        B["xs"] = [Buf(ar.alloc("xs%d" % i, [P, D], F32), "xs%d" % i) for i in range(3)]
        B["xh"] = [Buf(ar.alloc("xh%d" % i, [P, D], BF16), "xh%d" % i) for i in range(2)]
        B["sg"] = [Buf(ar.alloc("sg%d" % i, [P, 512], BF16), "sg%d" % i) for i in range(2)]
        B["ho"] = [Buf(ar.alloc("ho%d" % i, [P, D], F32), "ho%d" % i) for i in range(2)]
        B["G"] = [Buf(ar.alloc("G%d" % i, [P, D], F32), "G%d" % i) for i in range(2)]
        B["junk"] = [Buf(ar.alloc("junk%d" % i, [P, D], BF16), "junk%d" % i) for i in range(2)]
        B["qks"] = [Buf(ar.alloc("qks%d" % i, [P, 512], BF16), "qks%d" % i) for i in range(3)]
        B["vst"] = [Buf(ar.alloc("vst%d" % i, [P, 4, 65], BF16), "vst%d" % i) for i in range(2)]
        B["vbst"] = [Buf(ar.alloc("vbst%d" % i, [P, 256], BF16), "vbst%d" % i) for i in range(2)]
        B["qis"] = [Buf(ar.alloc("qis%d" % i, [P, 512], F32), "qis%d" % i) for i in range(2)]
        B["qiTs"] = [Buf(ar.alloc("qiTs%d" % i, [P, 4, P], F32), "qiTs%d" % i) for i in range(2)]
        B["kis"] = [Buf(ar.alloc("kis%d" % i, [P, 64], F32), "kis%d" % i) for i in range(2)]
        B["kiTs"] = [Buf(ar.alloc("kiTs%d" % i, [64, P], F32), "kiTs%d" % i) for i in range(2)]
        B["wabs"] = [Buf(ar.alloc("wabs%d" % i, [P, 8], F32), "wabs%d" % i) for i in range(2)]
        B["sgn"] = [Buf(ar.alloc("sgn%d" % i, [P, 8], F32), "sgn%d" % i) for i in range(2)]
        B["wkw"] = Buf(ar.alloc("wkw", [P, KC, 72], BF16), "wkw")
        B["kwsb"] = [Buf(ar.alloc("kwsb%d" % i, [P, 72], F32), "kwsb%d" % i) for i in range(2)]
        B["cnt"] = {}
        return B

    ctr = {}

    def rot(key, lst):
        ctr[key] = ctr.get(key, -1) + 1
        return lst[ctr[key] % len(lst)]

    def rstd_from_ss(ss_ap, ss_t, inv_n, n=1):
        v = small()
        r = small()
        p.op("pool", lambda e: e.tensor_scalar(v.ap[:, 0:n], ss_ap, inv_n, EPS, ALU.mult, ALU.add),
             reads=[ss_t], writes=[v.t])
        p.op("pool", lambda e: e.tensor_tensor(r.ap[:, 0:n], v.ap[:, 0:n],
                                               neghalf.ap.to_broadcast([P, n]) if n > 1 else neghalf.ap,
                                               ALU.pow),
             reads=[v.t, neghalf.t], writes=[r.t])
        return r

    def prenorm_T(B, src, groups, acol, scol, dstT, col0, tbank):
        ng = len(groups)
        ss = small()
        for gi, (c0, c1) in enumerate(groups):
            jk = rot("junk", B["junk"])
            p.op("act", lambda e, gi=gi, c0=c0, c1=c1, jk=jk: e.activation(
                jk.ap[:, c0:c1], src.ap[:, c0:c1], AF.Square, accum_out=ss.ap[:, gi:gi + 1]),
                reads=[src.t], writes=[ss.t, jk.t])
        r = rstd_from_ss(ss.ap[:, 0:ng], ss.t, 1.0 / (groups[0][1] - groups[0][0]), ng)
        xh = rot("xh", B["xh"])
        for gi, (c0, c1) in enumerate(groups):
            p.op("dve", lambda e, gi=gi, c0=c0, c1=c1: e.tensor_scalar(
                xh.ap[:, c0:c1], src.ap[:, c0:c1], r.ap[:, gi:gi + 1], None, ALU.mult),
                reads=[src.t, r.t], writes=[xh.t])
        pst = pbf(tbank).rearrange("p (k c) -> p k c", k=KC)

        def fn_t(e):
            rr = None
            for k in range(KC):
                rr = e.transpose(pst[:, k, :], xh.ap[:, k * P:(k + 1) * P], identb.ap)
            return rr
        p.op("pe", fn_t, reads=[xh.t, identb.t], writes=[pb[tbank]])
        for k in range(KC):
            if scol is not None:
                p.op("dve", lambda e, k=k: e.tensor_scalar(
                    dstT.ap[:, k, col0:col0 + P], pst[:, k, :], acol[:, k:k + 1], scol[:, k:k + 1],
                    ALU.mult, ALU.add), reads=[pb[tbank], Acol.t, Scol.t], writes=[dstT.t])
            else:
                p.op("dve", lambda e, k=k: e.tensor_scalar(
                    dstT.ap[:, k, col0:col0 + P], pst[:, k, :], acol[:, k:k + 1], None, ALU.mult),
                    reads=[pb[tbank], goutT.t], writes=[dstT.t])

    def load_G(B, b, i):
        G = rot("G", B["G"])
        p.dma("sp", lambda e: e.dma_start(out=G.ap, in_=grow_s[b, i, :].partition_broadcast(P)), G.t,
              reads=[T_grow], writes=[G.t])
        return G

    def post_residual(B, yb, G, res_ap, res_tiles, dst_ap, dst_tile, nxt):
        yv = ps[:, yb:yb + 2, :].rearrange("p a c -> p (a c)")
        ss = small()
        jk = rot("junk", B["junk"])
        p.op("act", lambda e: e.activation(jk.ap, yv, AF.Square, accum_out=ss.ap[:, 0:1]),
             reads=[pb[yb], pb[yb + 1]], writes=[ss.t, jk.t])
        r = rstd_from_ss(ss.ap[:, 0:1], ss.t, 1.0 / D)
        xs = rot("xs", B["xs"])
        p.dma("sp", lambda e: e.dma_start(out=xs.ap, in_=res_ap), xs.t, reads=res_tiles, writes=[xs.t])
        ho = rot("ho", B["ho"])
        p.op("dve", lambda e: e.scalar_tensor_tensor(ho.ap, yv, r.ap[:, 0:1], G.ap, ALU.mult, ALU.mult),
             reads=[pb[yb], pb[yb + 1], r.t, G.t], writes=[ho.t])
        p.op("dve", lambda e: e.tensor_tensor(ho.ap, ho.ap, xs.ap, ALU.add), reads=[xs.t], writes=[ho.t])
        p.dma("sp", lambda e: e.dma_start(out=dst_ap, in_=ho.ap), ho.t, reads=[ho.t], writes=[dst_tile])
        if nxt is not None:
            acol, scol, dstT, col0, tbank = nxt
            prenorm_T(B, ho, [(0, D)], acol, scol, dstT, col0, tbank)

    def load_wd(B, f):
        for i in range(2):
            wd = B["wd"][i]
            p.dma("pool", lambda e, wd=wd, i=i: e.dma_start(out=wd.ap, in_=wd_d[f][:, i * 11:(i + 1) * 11, :]),
                  wd.t, writes=[wd.t])

    def load_wgu(B, f, ffc):
        wr = rot("wr", B["wr"])
        p.dma("pool", lambda e: e.dma_start(out=wr.ap.rearrange("p a k c -> p a (k c)"),
                                            in_=wgu_d[f][ffc].rearrange("p a k c -> p a (k c)")),
              wr.t, writes=[wr.t])
        return wr

    def gate_up(B, f, nT):
        load_wd(B, f)
        PF = 2
        slots = {}
        for c in range(min(PF, NFF)):
            slots[c] = load_wgu(B, f, c)
        for ffc in range(NFF):
            if ffc + PF < NFF:
                slots[ffc + PF] = load_wgu(B, f, ffc + PF)
            wr = slots.pop(ffc)
            for half in range(2):
                idx = ffc * 2 + half
                gb, ub = idx % 2, 2 + idx % 2
                cs = slice(half * 512, (half + 1) * 512)

                def fn_g(e, wr=wr, gb=gb, cs=cs, a=0):
                    r = None
                    for k in range(KC):
                        r = e.matmul(ps[:, gb, :], wr.ap[:, a, k, :], nT.ap[:, k, cs], start=(k == 0),
                                     stop=(k == KC - 1))
                    return r

                def fn_u(e, wr=wr, ub=ub, cs=cs):
                    r = None
                    for k in range(KC):
                        r = e.matmul(ps[:, ub, :], wr.ap[:, 1, k, :], nT.ap[:, k, cs], start=(k == 0),
                                     stop=(k == KC - 1))
                    return r
                p.op("pe", fn_g, reads=[wr.t, nT.t], writes=[pb[gb]])
                p.op("pe", fn_u, reads=[wr.t, nT.t], writes=[pb[ub]])
                sg = rot("sg", B["sg"])
                p.op("act", lambda e, sg=sg, gb=gb: e.activation(sg.ap, ps[:, gb, :], AF.Silu),
                     reads=[pb[gb]], writes=[sg.t])
                p.op("dve", lambda e, sg=sg, ub=ub, ffc=ffc, cs=cs: e.tensor_tensor(
                    B["hT_ap"][:, ffc, cs], sg.ap, ps[:, ub, :], ALU.mult),
                    reads=[sg.t, pb[ub]], writes=[B["hT"][ffc][half]])

    def down(B, s, yb):
        th = s // 4
        for half in range(2):
            def fn(e, half=half):
                r = None
                for c in range(NFF):
                    wd = B["wd"][c // 11]
                    r = e.matmul(ps[:, yb + half, :], B["hT_ap"][:, c, s * P:(s + 1) * P],
                                 wd.ap[:, c % 11, half * 512:(half + 1) * 512], start=(c == 0), stop=(c == NFF - 1))
                return r
            p.op("pe", fn, reads=[B["hT"][c][th] for c in range(NFF)] + [B["wd"][0].t, B["wd"][1].t],
                 writes=[pb[yb + half]])

    def project(B, b, ti, nT):
        t0 = ti * TT
        for c2 in range(8):
            wr = rot("wr", B["wr"])
            p.dma("pool", lambda e, wr=wr, c2=c2: e.dma_start(
                out=wr.ap.rearrange("p a k c -> p a (k c)"), in_=winfm_d[c2].rearrange("p a k c -> p a (k c)")),
                wr.t, writes=[wr.t])
            for a in range(2):
                ch = c2 * 2 + a
                kind, pair = ch // 4, ch % 4
                scale = 0.125 if kind in (0, 2) else 1.0
                for half in range(2):
                    bk = rot("pjb", [0, 1, 2, 3])
                    cs = slice(half * 512, (half + 1) * 512)

                    def fn(e, wr=wr, a=a, bk=bk, cs=cs):
                        r = None
                        for k in range(KC):
                            r = e.matmul(ps[:, bk, :], wr.ap[:, a, k, :], nT.ap[:, k, cs], start=(k == 0),
                                         stop=(k == KC - 1))
                        return r
                    p.op("pe", fn, reads=[wr.t, nT.t], writes=[pb[bk]])
                    st = rot("qks", B["qks"])
                    p.op("act", lambda e, st=st, bk=bk, scale=scale: e.activation(
                        st.ap, ps[:, bk, :], AF.Copy, scale=scale), reads=[pb[bk]], writes=[st.t])
                    p.dma("sp", lambda e, st=st, kind=kind, pair=pair, half=half: e.dma_start(
                        out=qkT_s[b, kind, pair, :, t0 + half * 512:t0 + (half + 1) * 512], in_=st.ap),
                        st.t, reads=[st.t], writes=[])
        for qv in range(4):
            wr = rot("wr", B["wr"])
            wv = wr.ap.rearrange("p a k c -> p (a k c)").rearrange("p (k c) -> p k c", k=KC)
            p.dma("pool", lambda e, wv=wv, wr=wr, qv=qv: e.dma_start(out=wv, in_=winv_d[qv]), wr.t,
                  writes=[wr.t])
            for s in range(NSUB):
                bk = rot("pjb", [0, 1, 2, 3])

                def fn(e, wv=wv, bk=bk, s=s):
                    r = None
                    for k in range(KC):
                        r = e.matmul(ps[:, bk, 0:256], nT.ap[:, k, s * P:(s + 1) * P], wv[:, k, :],
                                     start=(k == 0), stop=(k == KC - 1))
                    return r
                p.op("pe", fn, reads=[wr.t, nT.t], writes=[pb[bk]])
                rows = slice(t0 + s * P, t0 + (s + 1) * P)
                if qv < 2:
                    st = rot("vst", B["vst"])
                    p.op("act", lambda e, st=st, bk=bk: e.activation(
                        st.ap[:, :, 0:64], ps[:, bk, 0:256].rearrange("p (h d) -> p h d", h=4), AF.Copy),
                        reads=[pb[bk]], writes=[st.t])
                    p.dma("sp", lambda e, st=st, rows=rows, qv=qv: e.dma_start(
                        out=va_s[b, rows, qv * 4:(qv + 1) * 4, :], in_=st.ap), st.t, reads=[st.t],
                        writes=[])
                else:
                    st = rot("vbst", B["vbst"])
                    p.op("act", lambda e, st=st, bk=bk: e.activation(st.ap, ps[:, bk, 0:256], AF.Copy),
                         reads=[pb[bk]], writes=[st.t])
                    p.dma("sp", lambda e, st=st, rows=rows, qv=qv: e.dma_start(
                        out=vb_s[b, rows, (qv - 2) * 256:(qv - 1) * 256], in_=st.ap), st.t, reads=[st.t],
                        writes=[])
        if debug and "noidx" in debug.get("_variant", ""):
            return
        wq = []
        for hq in range(2):
            wr = rot("wr", B["wr"])
            wv = wr.ap.rearrange("p a k c -> p (a k c)").rearrange("p (k c) -> p k c", k=KC)
            p.dma("pool", lambda e, wv=wv, hq=hq: e.dma_start(out=wv, in_=winqi_d[hq]), wr.t, writes=[wr.t])
            wq.append((wr, wv))
        wkw = B["wkw"]
        p.dma("pool", lambda e: e.dma_start(out=wkw.ap, in_=winkw_d), wkw.t, writes=[wkw.t])
        for s in range(NSUB):
            rows = slice(t0 + s * P, t0 + (s + 1) * P)
            bq = rot("pjb", [0, 1, 2, 3])
            bkw = rot("pjb", [0, 1, 2, 3])

            def fn_q(e, bq=bq, s=s):
                r = None
                for hq in range(2):
                    for k in range(KC):
                        r = e.matmul(ps[:, bq, hq * 256:(hq + 1) * 256], nT.ap[:, k, s * P:(s + 1) * P],
                                     wq[hq][1][:, k, :], start=(k == 0), stop=(k == KC - 1))
                return r
            p.op("pe", fn_q, reads=[wq[0][0].t, wq[1][0].t, nT.t], writes=[pb[bq]])

            def fn_kw(e, bkw=bkw, s=s):
                r = None
                for k in range(KC):
                    r = e.matmul(ps[:, bkw, 0:72], nT.ap[:, k, s * P:(s + 1) * P], wkw.ap[:, k, :],
                                 start=(k == 0), stop=(k == KC - 1))
                return r
            p.op("pe", fn_kw, reads=[wkw.t, nT.t], writes=[pb[bkw]])
            kw = rot("kwsb", B["kwsb"])
            p.op("dve", lambda e, kw=kw, bkw=bkw: e.tensor_copy(kw.ap, ps[:, bkw, 0:72]), reads=[pb[bkw]], writes=[kw.t])
            wabs = rot("wabs", B["wabs"])
            p.op("act", lambda e, wabs=wabs, kw=kw: e.activation(
                wabs.ap, kw.ap[:, 64:72], AF.Abs, scale=512.0 ** -0.5),
                reads=[kw.t], writes=[wabs.t])
            qt_ = (t0 + s * P) // P
            sg_ap = sgn_all.ap[:, b, qt_, :]
            p.op("dve", lambda e, sg_ap=sg_ap, kw=kw: e.tensor_scalar(
                sg_ap, kw.ap[:, 64:72], 0.0, 0.5, ALU.is_gt, ALU.subtract), reads=[kw.t], writes=[sgn_all.t])
            p.op("dve", lambda e, sg_ap=sg_ap: e.tensor_scalar(sg_ap, sg_ap, 2.0, None, ALU.mult),
                 reads=[sgn_all.t], writes=[sgn_all.t])
            qis = rot("qis", B["qis"])
            p.op("dve", lambda e, qis=qis, bq=bq, wabs=wabs: e.tensor_tensor(
                qis.ap.rearrange("p (h d) -> p h d", h=8), ps[:, bq, :].rearrange("p (h d) -> p h d", h=8),
                wabs.ap.unsqueeze(2).to_broadcast([P, 8, 64]), ALU.mult),
                reads=[pb[bq], wabs.t], writes=[qis.t])
            ss = small()
            jk = rot("junk", B["junk"])
            p.op("act", lambda e, kw=kw, ss=ss, jk=jk: e.activation(
                jk.ap[:, 0:64], kw.ap[:, 0:64], AF.Square, accum_out=ss.ap[:, 0:1]),
                reads=[kw.t], writes=[ss.t, jk.t])
            r = rstd_from_ss(ss.ap[:, 0:1], ss.t, 1.0 / 64)
            kis = rot("kis", B["kis"])
            p.op("dve", lambda e, kis=kis, kw=kw, r=r: e.scalar_tensor_tensor(
                kis.ap, kw.ap[:, 0:64], r.ap[:, 0:1], gk.ap, ALU.mult, ALU.mult),
                reads=[kw.t, r.t, gk.t], writes=[kis.t])
            if debug and "notr" in debug.get("_variant", ""):
                continue
            bt = rot("pjb", [0, 1, 2, 3])

            def fn_tr(e, bt=bt, qis=qis, kis=kis):
                r2 = None
                for c in range(4):
                    r2 = e.transpose(ps[:, bt, c * P:(c + 1) * P], qis.ap[:, c * P:(c + 1) * P], identf.ap)
                return r2
            p.op("pe", fn_tr, reads=[qis.t, identf.t], writes=[pb[bt]])
            bt2 = rot("pjb", [0, 1, 2, 3])
            p.op("pe", lambda e, bt2=bt2, kis=kis: e.transpose(ps[0:64, bt2, 0:P], kis.ap, identf.ap),
                 reads=[kis.t, identf.t], writes=[pb[bt2]])
            qiTs = rot("qiTs", B["qiTs"])
            kiTs = rot("kiTs", B["kiTs"])
            p.op("act", lambda e, qiTs=qiTs, bt=bt: e.activation(
                qiTs.ap.rearrange("p c t -> p (c t)"), ps[:, bt, :], AF.Copy), reads=[pb[bt]], writes=[qiTs.t])
            p.op("act", lambda e, kiTs=kiTs, bt2=bt2: e.activation(kiTs.ap, ps[0:64, bt2, 0:P], AF.Copy),
                 reads=[pb[bt2]], writes=[kiTs.t])
            p.dma("sp", lambda e, qiTs=qiTs, rows=rows: e.dma_start(
                out=qiT_s[b, :, :, rows].rearrange("c p t -> p c t"), in_=qiTs.ap), qiTs.t, reads=[qiTs.t],
                writes=[])
            p.dma("sp", lambda e, kiTs=kiTs, rows=rows: e.dma_start(out=kiT_s[b, :, rows], in_=kiTs.ap),
                  kiTs.t, reads=[kiTs.t], writes=[])

    def attention_phase():
        ar.reset()
        kT = Buf(ar.alloc("kT", [P, 4, S], BF16), "kT")
        qT = Buf(ar.alloc("qT", [P, 4, S], BF16), "qT")
        vv = Buf(ar.alloc("vv", [P, 16, 520], BF16), "vv")
        qiT = Buf(ar.alloc("qiT", [P, 4, S], F32), "qiT")
        kiT = Buf(ar.alloc("kiT", [P, S], F32), "kiT")
        sgn = Buf(ar.alloc("sgnA", [P, 16, 8], F32), "sgnA")
        tbp = Buf(ar.alloc("tbp", [P, 2, 8, P], BF16), "tbp")
        trawf = Buf(ar.alloc("trawf", [P, 2, 8, P], F32), "trawf")
        sbmask = Buf(ar.alloc("sbmask", [P, 4, 512], BF16), "sbmask")
        Sb = [Buf(ar.alloc("Sb%d" % i, [P, S], F32), "Sb%d" % i) for i in range(2)]
        rl = [Buf(ar.alloc("rl%d" % i, [P, 512], F32), "rl%d" % i) for i in range(3)]
        NM = Buf(ar.alloc("NM", [P, S], BF16), "NM")
        cjunk = [Buf(ar.alloc("cjunk%d" % i, [P, S], BF16), "cjunk%d" % i) for i in range(2)]
        NMT = [Buf(ar.alloc("NMT%d" % i, [P, 16, P], BF16), "NMT%d" % i) for i in range(2)]
        PT = [Buf(ar.alloc("PT%d" % i, [P, 512], BF16), "PT%d" % i) for i in range(4)]
        oA = [Buf(ar.alloc("oA%d" % i, [P, 8, 64], F32), "oA%d" % i) for i in range(2)]
        steps = [Buf(ar.alloc("steps%d" % i, [P, NIT], F32), "steps%d" % i) for i in range(2)]
        mcol = Buf(ar.alloc("mcol", [P, 8], F32), "mcol")
        sqt = [Buf(ar.alloc("sqt%d" % i, [P, S], BF16), "sqt%d" % i) for i in range(2)]
        nrm = [Buf(ar.alloc("nrm%d" % i, [P, 8, 4], F32), "nrm%d" % i) for i in range(2)]
        eb = [Buf(ar.alloc("eb%d" % i, [P, 512], F32), "eb%d" % i) for i in range(4)]
        spb = [Buf(ar.alloc("spb%d" % i, [P, 512], F32), "spb%d" % i) for i in range(4)]
        Rb = [Buf(ar.alloc("Rb%d" % i, [P, 512], F32), "Rb%d" % i) for i in range(2)]
        aTb = [Buf(ar.alloc("aTb%d" % i, [P, 512], BF16), "aTb%d" % i) for i in range(2)]
        oB = Buf(ar.alloc("oB", [P, 4, 512], F32), "oB")
        poshalf = Buf(ar.alloc("poshalf", [P, 1], F32), "poshalf")
        thrb = [Buf(ar.alloc("thr%d" % i, [P, 1], F32), "thr%d" % i) for i in range(2)]
        if debug and debug.get("_report"):
            print("ATT arena used", ar.off, "of", ar.size)
        p.op("dve", lambda e: e.memset(poshalf.ap, 0.5), writes=[poshalf.t])
        for bf in NMT + PT + Sb + [NM] + rl:
            p.op("dve", lambda e, bf=bf: e.memset(bf.ap, 0.0), writes=[bf.t])

        ld(trawf, traw_d)
        ld(sbmask, sbmask_d)
        for h in range(8):
            p.op("dve", lambda e, h=h: e.tensor_scalar(tbp.ap[:, :, h, :], trawf.ap[:, :, h, :], b31.ap[:, h:h + 1],
                                                       None, ALU.subtract), reads=[trawf.t, b31.t], writes=[tbp.t])

        def ldr(buf, out_ap, src, eng="sp"):
            p.dma(eng, lambda e: e.dma_start(out=out_ap, in_=src), buf.t, writes=[buf.t])


        def load_dsa(b):
            ldr(kT, kT.ap, qkT_s[b, 1].rearrange("c p s -> p c s"))
            ldr(qT, qT.ap, qkT_s[b, 0].rearrange("c p s -> p c s"))
            ldr(vv, vv.ap, va_s[b].rearrange("(q p) h d -> p q (h d)", p=P))
            ldr(qiT, qiT.ap, qiT_s[b].rearrange("c p s -> p c s"))
            ldr(kiT, kiT.ap[0:64, :], kiT_s[b])
            ldr(kiT, kiT.ap[64:128, :], kiT_s[b])

        def norm_bound(which, src, pair):
            nr = nrm[which]
            sq = rot("sqt", sqt)
            p.op("dve", lambda e: e.tensor_tensor(sq.ap, src.ap[:, pair, :], src.ap[:, pair, :], ALU.mult),
                 reads=[src.t], writes=[sq.t])
            for half in range(2):
                h = pair * 2 + half
                for ch in range(4):
                    bk = rot("nb", [0, 1, 2, 3])
                    p.op("pe", lambda e, half=half, ch=ch, bk=bk: e.matmul(
                        ps[:, bk, :], selh.ap[:, half, :], sq.ap[:, ch * 512:(ch + 1) * 512], start=True, stop=True),
                        reads=[sq.t, selh.t], writes=[pb[bk]])
                    p.op("dve", lambda e, h=h, ch=ch, bk=bk: e.tensor_reduce(
                        nr.ap[:, h, ch:ch + 1], ps[:, bk, :], AX.X, ALU.max), reads=[pb[bk]], writes=[nr.t])

        def shift_cols():
            for which, src in enumerate((qT, kT)):
                for pair in range(4):
                    norm_bound(which, src, pair)
            n2q = small()
            n2k = small()
            p.op("dve", lambda e: e.tensor_reduce(n2q.ap, nrm[0].ap, AX.X, ALU.max), reads=[nrm[0].t], writes=[n2q.t])
            p.op("dve", lambda e: e.tensor_reduce(n2k.ap, nrm[1].ap, AX.X, ALU.max), reads=[nrm[1].t], writes=[n2k.t])
            m2 = small()
            p.op("dve", lambda e: e.tensor_tensor(m2.ap, n2q.ap, n2k.ap, ALU.mult), reads=[n2q.t, n2k.t], writes=[m2.t])
            mm_ = small()
            p.op("pool", lambda e: e.tensor_tensor(mm_.ap, m2.ap, poshalf.ap.to_broadcast([P, 8]), ALU.pow),
                 reads=[m2.t, poshalf.t], writes=[mm_.t])
            p.op("dve", lambda e: e.scalar_tensor_tensor(mcol.ap, mm_.ap, -1.01, b31.ap, ALU.mult, ALU.add),
                 reads=[mm_.t, b31.t], writes=[mcol.t])

        def idx_head(b, qi, Sq, h, N):
            pair, r0 = h // 2, 64 * (h % 2)
            nch = (N + 511) // 512
            for ch in range(nch):
                w = min(512, N - ch * 512)
                bk = rot("ib", [0, 1])
                p.op("pe", lambda e, ch=ch, w=w, bk=bk: e.matmul(
                    ps[:, bk, 0:w], qiT.ap[r0:r0 + 64, pair, qi * P:(qi + 1) * P],
                    kiT.ap[r0:r0 + 64, ch * 512:ch * 512 + w], start=True, stop=True),
                    reads=[qiT.t, kiT.t], writes=[pb[bk]])
                r = rot("rl", rl)
                p.op("act", lambda e, r=r, w=w, bk=bk: e.activation(r.ap[:, 0:w], ps[:, bk, 0:w], AF.Relu),
                     reads=[pb[bk]], writes=[r.t])
                cs = slice(ch * 512, ch * 512 + w)
                if h == 0:
                    p.op("dve", lambda e, r=r, w=w, cs=cs: e.tensor_scalar(
                        Sq.ap[:, cs], r.ap[:, 0:w], sgn_all.ap[:, b, qi, 0:1], None, ALU.mult),
                        reads=[r.t, sgn_all.t], writes=[Sq.t])
                else:
                    p.op("dve", lambda e, r=r, w=w, cs=cs: e.scalar_tensor_tensor(
                        Sq.ap[:, cs], r.ap[:, 0:w], sgn_all.ap[:, b, qi, h:h + 1], Sq.ap[:, cs], ALU.mult, ALU.add),
                        reads=[r.t, sgn_all.t], writes=[Sq.t])

        def bisect_iter(Sq, stp, lo, it, N):
            mid = small()
            cnt = small()
            dl = small()
            lo2 = small()
            p.op("dve", lambda e: e.tensor_tensor(mid.ap[:, 0:1], lo.ap[:, 0:1], stp.ap[:, it:it + 1], ALU.add),
                 reads=[lo.t, stp.t], writes=[mid.t])
            cj = rot("cjunk", cjunk)
            p.op("dve", lambda e: e.tensor_scalar(cj.ap[:, 0:N], Sq.ap[:, 0:N], mid.ap[:, 0:1], None, ALU.is_ge, ALU.add,
                                                  accum_out=cnt.ap[:, 0:1]), reads=[Sq.t, mid.t], writes=[cnt.t, cj.t])
            p.op("dve", lambda e: e.scalar_tensor_tensor(dl.ap[:, 0:1], cnt.ap[:, 0:1], TOPK - 0.5, stp.ap[:, it:it + 1],
                                                         ALU.is_ge, ALU.mult), reads=[cnt.t, stp.t], writes=[dl.t])
            p.op("dve", lambda e: e.tensor_tensor(lo2.ap[:, 0:1], lo.ap[:, 0:1], dl.ap[:, 0:1], ALU.add),
                 reads=[lo.t, dl.t], writes=[lo2.t])
            return lo2

        selst = {}

        def dsa_selA(b, qi):
            N = P * (qi + 1)
            if qi < 2:
                return
            Sq = Sb[qi % 2]
            for h in range(8):
                idx_head(b, qi, Sq, h, N)
            bm = small()
            p.op("dve", lambda e: e.tensor_reduce(bm.ap[:, 0:1], Sq.ap[:, 0:N], AX.X, ALU.max,
                                                  apply_absolute_value=True), reads=[Sq.t], writes=[bm.t])
            p.op("dve", lambda e: e.tensor_tensor(Sq.ap[:, N - P:N], Sq.ap[:, N - P:N], negdiag.ap, ALU.add),
                 reads=[negdiag.t], writes=[Sq.t])
            stp = steps[qi % 2]
            bm2 = small()
            p.op("dve", lambda e: e.tensor_scalar(bm2.ap[:, 0:1], bm.ap[:, 0:1], 2.0, 1e-30, ALU.mult, ALU.add),
                 reads=[bm.t], writes=[bm2.t])
            p.op("dve", lambda e: e.tensor_scalar(stp.ap, pow2.ap, bm2.ap[:, 0:1], None, ALU.mult),
                 reads=[pow2.t, bm2.t], writes=[stp.t])
            lo = small()
            p.op("dve", lambda e, lo=lo: e.tensor_scalar(lo.ap[:, 0:1], bm.ap[:, 0:1], -1.0, None, ALU.mult),
                 reads=[bm.t], writes=[lo.t])
            for it in range(NIT):
                lo = bisect_iter(Sq, stp, lo, it, N)
            thr = thrb[qi % 2]
            p.op("dve", lambda e, lo=lo: e.tensor_copy(thr.ap, lo.ap[:, 0:1]), reads=[lo.t], writes=[thr.t])

        def dsa_selB(qi, nmt):
            N = P * (qi + 1)
            if qi < 2:
                if qi == 1:
                    p.op("dve", lambda e: e.memset(nmt.ap[:, 0, :], 0.0), writes=[nmt.t])
                p.op("dve", lambda e: e.tensor_copy(nmt.ap[:, qi, :], nmtc.ap), reads=[nmtc.t], writes=[nmt.t])
                return
            Sq = Sb[qi % 2]
            thr = thrb[qi % 2]
            p.op("dve", lambda e: e.tensor_scalar(NM.ap[:, 0:N], Sq.ap[:, 0:N], thr.ap[:, 0:1], NEG, ALU.is_lt, ALU.mult),
                 reads=[Sq.t, thr.t], writes=[NM.t])
            nmps = ps[:, 2:4, :].rearrange("p a c -> p (a c)").bitcast(BF16).rearrange("p (k c) -> p k c", c=P)

            def fn_nt(e):
                r2 = None
                for blk in range(qi + 1):
                    r2 = e.transpose(nmps[:, blk, :], NM.ap[:, blk * P:(blk + 1) * P], identb.ap)
                return r2
            p.op("pe", fn_nt, reads=[NM.t, identb.t], writes=[pb[2], pb[3]])
            p.op("act", lambda e: e.activation(nmt.ap[:, 0:qi + 1, :], nmps[:, 0:qi + 1, :], AF.Copy),
                 reads=[pb[2], pb[3]], writes=[nmt.t])

        def dsa_L(qi, nmt, h, g0):
            pair, r0 = h // 2, 64 * (h % 2)
            nb = min(4, qi + 1 - g0)
            bk = rot("lb", [4, 5])

            def fn_l(e):
                r2 = None
                for j in range(nb):
                    blk = g0 + j
                    near = blk >= qi - 1
                    o_ = ps[:, bk, j * P:(j + 1) * P]
                    e.matmul(o_, kT.ap[r0:r0 + 64, pair, blk * P:(blk + 1) * P],
                             qT.ap[r0:r0 + 64, pair, qi * P:(qi + 1) * P], start=True, stop=False)
                    r2 = e.matmul(o_, identb.ap, nmt.ap[:, blk, :], start=False, stop=not near)
                    if near:
                        r2 = e.matmul(o_, identb.ap, tbp.ap[:, qi - blk, h, :], start=False, stop=True)
                return r2
            p.op("pe", fn_l, reads=[kT.t, qT.t, nmt.t, identb.t, tbp.t], writes=[pb[bk]])
            pt = rot("PT", PT)
            p.op("act", lambda e: e.activation(
                pt.ap[:, 0:nb * P], ps[:, bk, 0:nb * P], AF.Exp, bias=mcol.ap[:, h:h + 1], scale=1.0),
                reads=[pb[bk], mcol.t], writes=[pt.t])
            return pt

        def dsa_AV(qi, h, g0, pt):
            ob = 6 + h // 4
            oc = (h % 4) * 65
            nb = min(4, qi + 1 - g0)

            def fn_av(e):
                r2 = None
                for j in range(nb):
                    blk = g0 + j
                    r2 = e.matmul(ps[:, ob, oc:oc + 65], pt.ap[:, j * P:(j + 1) * P],
                                  vv.ap[:, blk, h * 65:(h + 1) * 65], start=(blk == 0), stop=(blk == qi))
                return r2
            p.op("pe", fn_av, reads=[pt.t, vv.t], writes=[pb[ob]])

        def dsa_heads(qi, nmt):
            seq = [(h, g0) for h in range(8) for g0 in range(0, qi + 1, 4)]
            pts = {0: dsa_L(qi, nmt, seq[0][0], seq[0][1])}
            for i, (h, g0) in enumerate(seq):
                if i + 1 < len(seq):
                    pts[i + 1] = dsa_L(qi, nmt, seq[i + 1][0], seq[i + 1][1])
                dsa_AV(qi, h, g0, pts.pop(i))

        def dsa_out(b, qi):
            o_ = rot("oA", oA)
            for hb, ob in enumerate((6, 7)):
                ov = ps[:, ob, 0:260].rearrange("p (h d) -> p h d", d=65)
                rc = small()
                p.op("dve", lambda e, rc=rc, ov=ov: e.reciprocal(rc.ap[:, 0:4], ov[:, :, 64]), reads=[pb[ob]], writes=[rc.t])
                p.op("dve", lambda e, rc=rc, ov=ov, hb=hb: e.tensor_tensor(
                    o_.ap[:, hb * 4:(hb + 1) * 4, :], ov[:, :, 0:64], rc.ap[:, 0:4].unsqueeze(2).to_broadcast([P, 4, 64]),
                    ALU.mult), reads=[pb[ob], rc.t], writes=[o_.t])
            p.dma("sp", lambda e: e.dma_start(out=o_s[b, qi * P:(qi + 1) * P, 0:512],
                                              in_=o_.ap.rearrange("p h d -> p (h d)")), o_.t, reads=[o_.t])

        vB = vv.ap[:, :, 0:512]

        def load_sb(b):
            ldr(kT, kT.ap, qkT_s[b, 3].rearrange("c p s -> p c s"))
            ldr(qT, qT.ap, qkT_s[b, 2].rearrange("c p s -> p c s"))
            ldr(vv, vB, vb_s[b].rearrange("(q p) d -> p q d", p=P))

        def sb_A(g, pair, half, j):
            rr = j - 4 * g
            r0 = 64 * half
            A = half
            kk = kT.ap[r0:r0 + 64, pair, j * P:(j + 1) * P]
            qq = qT.ap[r0:r0 + 64, pair, g * 512:(g + 1) * 512]

            def fn_a(e):
                r2 = e.matmul(ps[:, A, :], kk, qq, start=True, stop=(rr < 0))
                if rr >= 0:
                    r2 = e.matmul(ps[:, A, :], identb.ap, sbmask.ap[:, rr, :], start=False, stop=True)
                return r2
            p.op("pe", fn_a, reads=[kT.t, qT.t, identb.t, sbmask.t], writes=[pb[A]])
            ebuf, sp = eb[half * 2 + j % 2], spb[half * 2 + j % 2]
            p.op("act", lambda e: e.activation(ebuf.ap, ps[:, A, :], AF.Exp), reads=[pb[A]], writes=[ebuf.t])
            p.op("act", lambda e: e.activation(sp.ap, ebuf.ap, AF.Ln, bias=1.0, scale=1.0), reads=[ebuf.t], writes=[sp.t])

        def sb_B(g, pair, half, j):
            top = 4 * g + 3
            rr = j - 4 * g
            h = pair * 2 + half
            r0 = 64 * half
            Bk, O = 2 + half, 6 + half
            kk = kT.ap[r0:r0 + 64, pair, j * P:(j + 1) * P]
            qq = qT.ap[r0:r0 + 64, pair, g * 512:(g + 1) * 512]
            sp, R, aT = spb[half * 2 + j % 2], Rb[half], aTb[half]

            def fn_b(e):
                mms = [(kk, qq), (negtri.ap, sp.ap)]
                if j < top:
                    mms.append((negones.ap, R.ap))
                if rr >= 0:
                    mms.append((identb.ap, sbmask.ap[:, rr, :]))
                r2 = None
                for i, (l_, r_) in enumerate(mms):
                    r2 = e.matmul(ps[:, Bk, :], l_, r_, start=(i == 0), stop=(i == len(mms) - 1))
                return r2
            p.op("pe", fn_b, reads=[kT.t, qT.t, sp.t, R.t, negtri.t, negones.t, identb.t, sbmask.t], writes=[pb[Bk]])
            if j > 0:
                if j == top:
                    p.op("dve", lambda e: e.tensor_copy(R.ap, sp.ap), reads=[sp.t], writes=[R.t])
                else:
                    p.op("dve", lambda e: e.tensor_tensor(R.ap, R.ap, sp.ap, ALU.add), reads=[sp.t], writes=[R.t])
            p.op("act", lambda e: e.activation(aT.ap, ps[:, Bk, :], AF.Exp), reads=[pb[Bk]], writes=[aT.t])

            def fn_av2(e):
                r2 = None
                for tt in range(4):
                    if 4 * g + tt >= j:
                        r2 = e.matmul(ps[:, O, tt * 64:(tt + 1) * 64], aT.ap[:, tt * P:(tt + 1) * P],
                                      vB[:, j, h * 64:(h + 1) * 64], start=False, stop=False, skip_group_check=True)
                return r2
            p.op("pe", fn_av2, reads=[aT.t, vv.t], writes=[pb[O]])

        def sb_pair(g, pair):
            top = 4 * g + 3
            for half in range(2):
                p.op("pe", lambda e, half=half: e.matmul(ps[:, 6 + half, 0:256], zerosb.ap[:, 0:P], zerosb.ap[:, 0:256],
                                                         start=True, stop=True), reads=[zerosb.t], writes=[pb[6 + half]])
            for half in range(2):
                sb_A(g, pair, half, top)
            for j in range(top, -1, -1):
                if j - 1 >= 0:
                    for half in range(2):
                        sb_A(g, pair, half, j - 1)
                for half in range(2):
                    sb_B(g, pair, half, j)
            for half in range(2):
                h = pair * 2 + half
                p.op("act", lambda e, half=half, h=h: e.activation(
                    oB.ap[:, :, h * 64:(h + 1) * 64], ps[:, 6 + half, 0:256].rearrange("p (t d) -> p t d", d=64), AF.Copy),
                    reads=[pb[6 + half]], writes=[oB.t])

        def sb_group(b, g):
            for pair in range(4):
                sb_pair(g, pair)
            p.dma("sp", lambda e: e.dma_start(
                out=o_s[b, g * 512:(g + 1) * 512, 512:1024].rearrange("(t p) d -> p t d", p=P), in_=oB.ap),
                oB.t, reads=[oB.t])

        for b in range(NBC if not (debug and debug.get("_b1")) else 1):
            load_dsa(b)
            shift_cols()
            dsa_selA(b, 0)
            dsa_selB(0, NMT[0])
            for qi in range(16):
                if qi + 1 < 16:
                    dsa_selA(b, qi + 1)
                dsa_heads(qi, NMT[qi % 2])
                dsa_out(b, qi)
                if qi + 1 < 16:
                    dsa_selB(qi + 1, NMT[(qi + 1) % 2])
            load_sb(b)
            for g in range(4):
                sb_group(b, g)

    def phase3():
        B = ffn_arena()
        wout_ap = B["wd"][1].ap[:, 0:8, :]
        wout_t = B["wd"][1].t
        p.op("dve", lambda e: e.memset(B["wd"][1].ap, 0.0), writes=[wout_t])
        tiles3 = [(b, ti) for b in range(NBC) for ti in range(NTILE)]
        if debug and debug.get("_b1"):
            tiles3 = tiles3[:NTILE]

        def pro3(b, ti, s, dstT):
            xs = rot("xs", B["xs"])
            rows = slice(ti * TT + s * P, ti * TT + (s + 1) * P)
            p.dma("sp", lambda e: e.dma_start(out=xs.ap, in_=o_s[b, rows, :]), xs.t, writes=[xs.t])
            prenorm_T(B, xs, [(0, 512), (512, D)], goutT.ap, None, dstT, s * P, 0)

        onT = rot("nT", B["nT"])
        for s in range(NSUB):
            pro3(tiles3[0][0], tiles3[0][1], s, onT)
        for idx, (b, ti) in enumerate(tiles3):
            G2 = load_G(B, b, 1)
            G3 = load_G(B, b, 2)
            p.dma("pool", lambda e: e.dma_start(out=wout_ap, in_=wout_d), wout_t, writes=[wout_t])
            n3T = rot("nT", B["nT"])
            pend = None
            for s in range(NSUB):
                yb = 4 + 2 * (s % 2)
                for half in range(2):
                    def fn(e, half=half, s=s, yb=yb, onT=onT):
                        r = None
                        for k in range(KC):
                            r = e.matmul(ps[:, yb + half, :], onT.ap[:, k, s * P:(s + 1) * P],
                                         wout_ap[:, k, half * 512:(half + 1) * 512], start=(k == 0), stop=(k == KC - 1))
                        return r
                    p.op("pe", fn, reads=[onT.t, wout_t], writes=[pb[yb + half]])
                if pend is not None:
                    pend()
                r0 = ti * TT + s * P
                rows = slice(r0, r0 + P)

                def epi(s=s, yb=yb, rows=rows, r0=r0):
                    post_residual(B, yb, G2, h1_s[b, rows, :], [], h2_s[b, rows, :], T_h2[b][r0 // P],
                                  (Acol.ap[:, 2, b, :], Scol.ap[:, 2, b, :], n3T, s * P, 1))
                pend = epi
            pend()
            gate_up(B, 1, n3T)
            nxt = tiles3[idx + 1] if idx + 1 < len(tiles3) else None
            if nxt is not None:
                onT = rot("nT", B["nT"])
            pend = None
            for s in range(NSUB):
                yb = 4 + 2 * (s % 2)
                down(B, s, yb)
                if nxt is not None:
                    pro3(nxt[0], nxt[1], s, onT)
                if pend is not None:
                    pend()
                r0 = ti * TT + s * P
                rows = slice(r0, r0 + P)

                def epi2(s=s, yb=yb, rows=rows, r0=r0):
                    post_residual(B, yb, G3, h2_s[b, rows, :], [T_h2[b][r0 // P]], out_d[b, rows, :], Tl("o"), None)
                pend = epi2
            pend()

    B = ffn_arena()
    for st in B["vst"]:
        p.op("dve", lambda e, st=st: e.memset(st.ap, 1.0), writes=[st.t])

    def prologue_sub(B, b, ti, s, dstT):
        xs = rot("xs", B["xs"])
        rows = slice(ti * TT + s * P, ti * TT + (s + 1) * P)
        p.dma("sp", lambda e: e.dma_start(out=xs.ap, in_=x_d[b, rows, :]), xs.t, writes=[xs.t])
        prenorm_T(B, xs, [(0, D)], Acol.ap[:, 0, b, :], Scol.ap[:, 0, b, :], dstT, s * P, 0)

    tiles = [(b, ti) for b in range(NBC) for ti in range(NTILE)]
    if debug and "_ntiles" in debug:
        tiles = tiles[:debug["_ntiles"]]
    if debug and "_tiles" in debug:
        tiles = debug["_tiles"]
    nT_cur = rot("nT", B["nT"])
    for s in range(NSUB):
        prologue_sub(B, tiles[0][0], tiles[0][1], s, nT_cur)
    for idx, (b, ti) in enumerate(tiles):
        G = load_G(B, b, 0)
        gate_up(B, 0, nT_cur)
        n2T = rot("nT", B["nT"])
        nxt = tiles[idx + 1] if idx + 1 < len(tiles) else None
        pend = None
        for s in range(NSUB):
            yb = 4 + 2 * (s % 2)
            down(B, s, yb)
            if pend is not None:
                pend()
            r0 = ti * TT + s * P
            rows = slice(r0, r0 + P)

            def epi(s=s, yb=yb, rows=rows, r0=r0):
                post_residual(B, yb, G, x_d[b, rows, :], [], h1_s[b, rows, :], Tl('h1'),
                              (Acol.ap[:, 1, b, :], Scol.ap[:, 1, b, :], n2T, s * P, 1))
            pend = epi
        pend()
        if not (debug and "noproj" in debug.get("_variant", "")):
            project(B, b, ti, n2T)
        if nxt is not None:
            nT_cur = rot("nT", B["nT"])
            for s in range(NSUB):
                prologue_sub(B, nxt[0], nxt[1], s, nT_cur)
    p.barrier()


    stop_at = debug.get("_stop", 99) if debug else 99
    if stop_at >= 2:
        attention_phase()
        p.barrier()
    if stop_at >= 3:
        phase3()
        p.barrier()

    p.barrier()
    with nc.Block() as block:
        @block.tensor
        def _(e):
            p.run(e, "pe")

        @block.scalar
        def _(e):
            p.run(e, "act")

        @block.vector
        def _(e):
            p.run(e, "dve")

        @block.gpsimd
        def _(e):
            p.run(e, "pool")

        @block.sync
        def _(e):
            p.run(e, "sp")
    return nc, stack, p


def _t5_bucket_np(n):
    n = np.maximum(n, 0)
    max_exact = 16
    nf = np.maximum(n, 1).astype(np.float32)
    large = max_exact + (np.log(nf / max_exact) / np.log(128 / max_exact) * (32 - max_exact)).astype(np.int32)
    large = np.minimum(large, 31)
    return np.where(n < max_exact, n, large)


def host_consts():
    bf = ml_dtypes.bfloat16
    c = {}
    c["identb"] = np.eye(P, dtype=np.float32).astype(bf)
    c["identf"] = np.eye(P, dtype=np.float32)
    jj = np.arange(P)[:, None]
    ss = np.arange(P)[None, :]
    c["negtri"] = np.where(jj >= ss, -1.0, 0.0).astype(np.float32)
    c["nmtc"] = np.where(jj > ss, NEG, 0.0).astype(np.float32).astype(bf)
    c["negdiag"] = np.where(ss > jj, -1e30, 0.0).astype(np.float32)
    t512 = np.arange(512)[None, None, :]
    r4 = np.arange(4)[None, :, None]
    s128 = np.arange(P)[:, None, None]
    c["sbmask"] = np.where(r4 * P + s128 < t512, 0.0, NEG).astype(np.float32).astype(bf)
    c["pow2"] = np.broadcast_to((2.0 ** -(np.arange(NIT) + 1.0)).astype(np.float32)[None, :], (P, NIT)).copy()
    selb = np.zeros((2, 2, P), np.float32)
    selb[0, 0, :] = 1.0
    selb[1, 1, :] = 1.0
    c["selb"] = selb
    selh = np.zeros((P, 2, P), np.float32)
    selh[:64, 0, :] = 1.0
    selh[64:, 1, :] = 1.0
    c["selh"] = selh.astype(bf)
    return c


def host_shared(inp):
    f = np.float32
    sh = {}
    w_ada = np.asarray(inp["w_ada"], f)[0]
    sh["wada"] = np.ascontiguousarray(w_ada.reshape(KC, P, 18, 512).transpose(2, 1, 0, 3))
    sh["bada"] = np.ascontiguousarray(np.asarray(inp["b_ada"], f)[0])
    sh["gpreT"] = np.ascontiguousarray(np.asarray(inp["g_pre"], f)[0].reshape(3, KC, P).transpose(2, 0, 1))
    sh["gpost"] = np.ascontiguousarray(np.asarray(inp["g_post"], f)[0])
    for ff in range(2):
        wg = np.asarray(inp["w_ffn_gate"], f)[0, ff].reshape(KC, P, NFF, P)
        wu = np.asarray(inp["w_ffn_up"], f)[0, ff].reshape(KC, P, NFF, P)
        gu = np.stack([wg, wu], axis=0)
        sh["wgu%d" % ff] = np.ascontiguousarray(gu.transpose(3, 2, 0, 1, 4))
        wd = np.asarray(inp["w_ffn_down"], f)[0, ff].reshape(NFF, P, D)
        sh["wd%d" % ff] = np.ascontiguousarray(wd.transpose(1, 0, 2))
    w_in = np.asarray(inp["w_in"], f)[0]
    qa, ka, va = w_in[:, 0:512], w_in[:, 512:1024], w_in[:, 1024:1536]
    qi, kiw = w_in[:, 1536:2048], w_in[:, 2048:2120]
    qb, kb, vb = w_in[:, 2120:2632], w_in[:, 2632:3144], w_in[:, 3144:3656]
    fm = np.concatenate([qa, ka, qb, kb], axis=1).reshape(KC, P, 8, 2, P)
    sh["winfm"] = np.ascontiguousarray(fm.transpose(2, 1, 3, 0, 4))
    vv = np.concatenate([va, vb], axis=1).reshape(KC, P, 4, 256)
    sh["winv"] = np.ascontiguousarray(vv.transpose(2, 1, 0, 3))
    sh["winqi"] = np.ascontiguousarray(qi.reshape(KC, P, 2, 256).transpose(2, 1, 0, 3))
    sh["winkw"] = np.ascontiguousarray(kiw.reshape(KC, P, 72).transpose(1, 0, 2))
    sh["wout"] = np.ascontiguousarray(np.asarray(inp["w_out"], f)[0].reshape(KC, P, D).transpose(1, 0, 2))
    sh["gk"] = np.ascontiguousarray(np.broadcast_to(np.asarray(inp["g_kidx"], f)[0][None, :], (P, 64)))
    gout = np.concatenate([np.asarray(inp["g_out_a"], f)[0], np.asarray(inp["g_out_b"], f)[0]])
    sh["goutT"] = np.ascontiguousarray(gout.reshape(KC, P).T)
    rb = np.asarray(inp["rel_bias"], f)
    s_ = np.arange(P)[:, None]
    t_ = np.arange(P)[None, :]
    tr = np.zeros((P, 2, 8, P), f)
    for v in range(2):
        bk = _t5_bucket_np(t_ - s_ + v * P)
        tr[:, v, :, :] = rb[bk].transpose(0, 2, 1)
    sh["traw"] = tr
    sh["b31"] = np.ascontiguousarray(np.broadcast_to(rb[31][None, :], (P, 8)))
    sh.update(host_consts())
    return sh


_CACHE = {}


def kernel(**inputs):
    x = np.asarray(inputs["x"], np.float32)
    c = np.asarray(inputs["c"], np.float32)
    sh = host_shared(inputs)
    if "nc" not in _CACHE:
        _CACHE["nc"] = build_program()
    nc, stack, prog = _CACHE["nc"]
    in_maps = []
    for i in range(N_CORES):
        m = dict(sh)
        m["x"] = np.ascontiguousarray(x[NBC * i:NBC * (i + 1)])
        m["cT"] = np.ascontiguousarray(c[NBC * i:NBC * (i + 1)].reshape(NBC, KC, P).transpose(2, 1, 0))
        in_maps.append(m)
    res = run_bass_kernel_spmd(nc, in_maps, core_ids=list(range(N_CORES)))
    out = np.concatenate([np.asarray(r["out"]) for r in res.results], axis=0)
    return out.astype(np.float32)
```
